# Optimizing a Trainium2 kernel written in Bass

```python
import math
import jax, jax.numpy as jnp
from jax import lax
import numpy as np

D_MODEL = 1024
BATCH = 4
SEQ = 8192
DEPTH = 1

MLA_HEADS = 8
QK_NOPE_DIM = 64
QK_ROPE_DIM = 32
QK_HEAD_DIM = QK_NOPE_DIM + QK_ROPE_DIM
V_HEAD_DIM = 64
Q_LORA_RANK = 256
KV_LORA_RANK = 128
ROPE_THETA = 10000.0
Q_BLOCK = 128
SSM_WIDTH = 512
SSM_GROUP = 16
SSM_GROUPS = SSM_WIDTH // SSM_GROUP
SSM_STATE = 64
DT_MIN = 1e-3
DT_MAX = 1e-1
N_BRANCHES = 2
PEER_HEADS = 8
N_KEYS = 128
N_EXPERTS = N_KEYS * N_KEYS
PEER_TOPK = 16
PEER_QUERY_DIM = 128
PEER_HALF = PEER_QUERY_DIM // 2
PEER_CHUNK = 128
EPS = 1e-6

IN_SPLITS = (Q_LORA_RANK,
             Q_LORA_RANK + KV_LORA_RANK,
             Q_LORA_RANK + KV_LORA_RANK + QK_ROPE_DIM,
             Q_LORA_RANK + KV_LORA_RANK + QK_ROPE_DIM + SSM_WIDTH)
IN_DIM = IN_SPLITS[-1] + N_BRANCHES * D_MODEL

kernel_name = "hybrid_mla_s5_peer_encoder"


def rmsnorm(x, g):
    xf = x.astype(jnp.float32)
    y = xf * lax.rsqrt(jnp.mean(xf * xf, axis=-1, keepdims=True) + EPS)
    return (y * g.astype(jnp.float32)).astype(x.dtype)


def rope(t, cos, sin):
    t1, t2 = jnp.split(t.astype(jnp.float32), 2, axis=-1)
    out = jnp.concatenate([t1 * cos - t2 * sin, t1 * sin + t2 * cos], axis=-1)
    return out.astype(t.dtype)


def mla(h_q, h_kv, k_r, q_a_norm, w_q_b, kv_a_norm, w_kv_b, cos, sin):
    B, S, _ = h_q.shape
    q = (rmsnorm(h_q, q_a_norm) @ w_q_b).reshape(B, S, MLA_HEADS, QK_HEAD_DIM)
    q_nope, q_rope = q[..., :QK_NOPE_DIM], q[..., QK_NOPE_DIM:]
    q_rope = rope(q_rope, cos[:, None, :], sin[:, None, :])
    kv = (rmsnorm(h_kv, kv_a_norm) @ w_kv_b).reshape(B, S, MLA_HEADS, QK_NOPE_DIM + V_HEAD_DIM)
    k_nope, v = kv[..., :QK_NOPE_DIM], kv[..., QK_NOPE_DIM:]
    k_rope = rope(k_r, cos, sin)
    k_rope = jnp.broadcast_to(k_rope[:, :, None, :], (B, S, MLA_HEADS, QK_ROPE_DIM))
    q = jnp.concatenate([q_nope, q_rope], axis=-1)
    k = jnp.concatenate([k_nope, k_rope], axis=-1)
    scale = QK_HEAD_DIM ** -0.5
    nb = S // Q_BLOCK
    qb = q.reshape(B, nb, Q_BLOCK, MLA_HEADS, QK_HEAD_DIM).transpose(1, 0, 2, 3, 4)

    def attend(q_blk):
        s = jnp.einsum('bqhd,bkhd->bhqk', q_blk, k).astype(jnp.float32) * scale
        p = jax.nn.softmax(s, axis=-1)
        return jnp.einsum('bhqk,bkhd->bqhd', p.astype(v.dtype), v)

    o = lax.map(attend, qb)
    return o.transpose(1, 0, 2, 3, 4).reshape(B, S, MLA_HEADS * V_HEAD_DIM)


def s5_scan(u, lam_re, lam_im, log_dt, b_re, b_im, c_re, c_im, reverse):
    S = u.shape[1]
    lam = lax.complex(lam_re.astype(jnp.float32), lam_im.astype(jnp.float32))
    dt = jnp.exp(log_dt.astype(jnp.float32))[:, None]
    lam_bar = jnp.exp(lam * dt)
    b_c = lax.complex(b_re.astype(jnp.float32), b_im.astype(jnp.float32))
    b_bar = ((lam_bar - 1.0) / lam)[..., None] * b_c
    c_c = lax.complex(c_re.astype(jnp.float32), c_im.astype(jnp.float32))
    bu = jnp.einsum('bsgh,gph->bsgp', u.astype(jnp.complex64), b_bar)
    if reverse:
        bu = jnp.flip(bu, axis=1)
    a = jnp.broadcast_to(lam_bar[None, None], (1, S) + lam_bar.shape)

    def combine(e1, e2):
        a1, b1 = e1
        a2, b2 = e2
        return a2 * a1, a2 * b1 + b2

    _, states = lax.associative_scan(combine, (a, bu), axis=1)
    if reverse:
        states = jnp.flip(states, axis=1)
    return jnp.einsum('bsgp,ghp->bsgh', states, c_c).real


def s5_bidir(u, lam_re, lam_im, log_dt, b_re, b_im, c_re, c_im, d_skip):
    B, S, _ = u.shape
    uf = u.astype(jnp.float32).reshape(B, S, SSM_GROUPS, SSM_GROUP)
    y = d_skip.astype(jnp.float32).reshape(SSM_GROUPS, SSM_GROUP) * uf
    for d in range(2):
        y = y + s5_scan(uf, lam_re[d], lam_im[d], log_dt[d], b_re[d], b_im[d],
                        c_re[d], c_im[d], reverse=(d == 1))
    return y.reshape(B, S, SSM_WIDTH).astype(u.dtype)


def peer(h, w_query, sub_keys, w_down, w_up):
    B, S, D = h.shape
    T = B * S
    hf = h.reshape(T, D)
    q = (hf @ w_query).reshape(T, PEER_HEADS, 2, PEER_HALF)
    scores = jnp.einsum('thnd,hnkd->thnk', q, sub_keys).astype(jnp.float32)
    s_top, i_top = lax.top_k(scores, PEER_TOPK)
    cand = s_top[:, :, 0, :, None] + s_top[:, :, 1, None, :]
    cand_idx = i_top[:, :, 0, :, None] * N_KEYS + i_top[:, :, 1, None, :]
    cand = cand.reshape(T, PEER_HEADS, PEER_TOPK * PEER_TOPK)
    cand_idx = cand_idx.reshape(T, PEER_HEADS, PEER_TOPK * PEER_TOPK)
    best, pos = lax.top_k(cand, PEER_TOPK)
    idx = jnp.take_along_axis(cand_idx, pos, axis=-1)
    gate = jax.nn.softmax(best, axis=-1)
    nc = T // PEER_CHUNK
    xc = hf.reshape(nc, PEER_CHUNK, D)
    ic = idx.reshape(nc, PEER_CHUNK, PEER_HEADS, PEER_TOPK)
    gc = gate.reshape(nc, PEER_CHUNK, PEER_HEADS, PEER_TOPK)

    def expert_chunk(args):
        xb, ib, gb = args
        u = w_down[ib]
        act = jax.nn.gelu(jnp.einsum('cd,chkd->chk', xb, u).astype(jnp.float32)) * gb
        v = w_up[ib]
        return jnp.einsum('chk,chkd->cd', act.astype(v.dtype), v)

    y = lax.map(expert_chunk, (xc, ic, gc))
    return y.reshape(B, S, D)


def setup_inputs(seed: int = 0) -> dict:
    key = jax.random.key(seed)
    ks = jax.random.split(key, 32)
    f32 = jnp.float32
    nrm = lambda k, shape, s: jax.random.normal(k, shape, f32) * s
    L, G, P, Hg = DEPTH, SSM_GROUPS, SSM_STATE, SSM_GROUP
    lam_im_base = jnp.pi * jnp.arange(P, dtype=f32)
    return {
        "x": nrm(ks[0], (BATCH, SEQ, D_MODEL), 1.0),
        "norm_mix": 1.0 + nrm(ks[1], (L, D_MODEL), 0.02),
        "w_in": nrm(ks[2], (L, D_MODEL, IN_DIM), D_MODEL ** -0.5),
        "q_a_norm": 1.0 + nrm(ks[3], (L, Q_LORA_RANK), 0.02),
        "w_q_b": nrm(ks[4], (L, Q_LORA_RANK, MLA_HEADS * QK_HEAD_DIM), Q_LORA_RANK ** -0.5),
        "kv_a_norm": 1.0 + nrm(ks[5], (L, KV_LORA_RANK), 0.02),
        "w_kv_b": nrm(ks[6], (L, KV_LORA_RANK, MLA_HEADS * (QK_NOPE_DIM + V_HEAD_DIM)), KV_LORA_RANK ** -0.5),
        "w_o_attn": nrm(ks[7], (L, MLA_HEADS * V_HEAD_DIM, D_MODEL), (MLA_HEADS * V_HEAD_DIM) ** -0.5),
        "lam_re": -0.5 * jnp.exp(nrm(ks[8], (L, 2, G, P), 0.05)),
        "lam_im": lam_im_base + nrm(ks[9], (L, 2, G, P), 0.01),
        "log_dt": jax.random.uniform(ks[10], (L, 2, G), f32, math.log(DT_MIN), math.log(DT_MAX)),
        "b_re": nrm(ks[11], (L, 2, G, P, Hg), (2 * Hg) ** -0.5),
        "b_im": nrm(ks[12], (L, 2, G, P, Hg), (2 * Hg) ** -0.5),
        "c_re": nrm(ks[13], (L, 2, G, Hg, P), 0.5),
        "c_im": nrm(ks[14], (L, 2, G, Hg, P), 0.5),
        "d_skip": nrm(ks[15], (L, SSM_WIDTH), 0.5),
        "w_glu": nrm(ks[16], (L, SSM_WIDTH, 2 * SSM_WIDTH), SSM_WIDTH ** -0.5),
        "w_o_ssm": nrm(ks[17], (L, SSM_WIDTH, D_MODEL), SSM_WIDTH ** -0.5),
        "w_out": nrm(ks[18], (L, D_MODEL, D_MODEL), D_MODEL ** -0.5),
        "norm_ffn": 1.0 + nrm(ks[19], (L, D_MODEL), 0.02),
        "w_query": nrm(ks[20], (L, D_MODEL, PEER_HEADS * PEER_QUERY_DIM), D_MODEL ** -0.5),
        "sub_keys": nrm(ks[21], (L, PEER_HEADS, 2, N_KEYS, PEER_HALF), PEER_HALF ** -0.5),
        "w_down": nrm(ks[22], (L, N_EXPERTS, D_MODEL), D_MODEL ** -0.5),
        "w_up": nrm(ks[23], (L, N_EXPERTS, D_MODEL), PEER_HEADS ** -0.5),
        "final_norm": 1.0 + nrm(ks[24], (D_MODEL,), 0.02),
    }


def reference(x, norm_mix, w_in, q_a_norm, w_q_b, kv_a_norm, w_kv_b, w_o_attn,
              lam_re, lam_im, log_dt, b_re, b_im, c_re, c_im, d_skip, w_glu, w_o_ssm,
              w_out, norm_ffn, w_query, sub_keys, w_down, w_up, final_norm):
    B, S, D = x.shape
    pos = jnp.arange(S, dtype=jnp.float32)
    inv_freq = 1.0 / (ROPE_THETA ** (jnp.arange(0, QK_ROPE_DIM, 2, dtype=jnp.float32) / QK_ROPE_DIM))
    ang = pos[:, None] * inv_freq[None, :]
    cos, sin = jnp.cos(ang), jnp.sin(ang)
    for l in range(DEPTH):
        h = rmsnorm(x, norm_mix[l])
        proj = h @ w_in[l]
        h_q, h_kv, k_r, u_ssm, gates = jnp.split(proj, IN_SPLITS, axis=-1)
        y_attn = mla(h_q, h_kv, k_r, q_a_norm[l], w_q_b[l], kv_a_norm[l], w_kv_b[l], cos, sin) @ w_o_attn[l]
        y_s = s5_bidir(u_ssm, lam_re[l], lam_im[l], log_dt[l], b_re[l], b_im[l],
                       c_re[l], c_im[l], d_skip[l])
        z = jax.nn.gelu(y_s) @ w_glu[l]
        y_ssm = (z[..., :SSM_WIDTH] * jax.nn.sigmoid(z[..., SSM_WIDTH:])) @ w_o_ssm[l]
        g = jax.nn.sigmoid(gates.astype(jnp.float32)).astype(x.dtype).reshape(B, S, N_BRANCHES, D)
        mixed = g[:, :, 0, :] * y_attn + g[:, :, 1, :] * y_ssm
        x = x + mixed @ w_out[l]
        x = x + peer(rmsnorm(x, norm_ffn[l]), w_query[l], sub_keys[l], w_down[l], w_up[l])
    return rmsnorm(x, final_norm)
```

```python
from contextlib import ExitStack
import numpy as np
import ml_dtypes
import concourse.bass as bass
import concourse.mybir as mybir
from concourse.bass_utils import run_bass_kernel_spmd

F32 = mybir.dt.float32
BF16 = mybir.dt.bfloat16
AF = mybir.ActivationFunctionType
ALU = mybir.AluOpType
AX = mybir.AxisListType
SEM_CAP = 30000
EPS = 1e-6


class Prog:
    ENGS = ("sync", "scalar", "vector", "gpsimd", "tensor")

    def __init__(self, nc):
        self.nc = nc
        self.ops = []

    def op(self, eng, fn, reads=(), writes=(), dma_key=None):
        xs = [k for k in reads if isinstance(k, tuple) and k and k[0] == "pb"]
        if xs:
            writes = tuple(writes) + tuple(k for k in xs if k not in writes)
        self.ops.append(dict(eng=eng, fn=fn, reads=tuple(reads), writes=tuple(writes),
                             dma_key=dma_key, waits=[], inc=None, idx=len(self.ops), bar=False))

    def dma(self, eng, fn, key, reads=(), writes=()):
        self.op(eng, fn, reads, writes, dma_key=key)

    def barrier(self):
        self.ops.append(dict(bar=True, idx=len(self.ops)))

    def analyze(self):
        last_w, readers = {}, {}
        eng_cnt = {e: 0 for e in self.ENGS}
        last_sig = {}
        dma_cnt = {}
        waited = {e: {} for e in self.ENGS}
        pending = {e: [] for e in self.ENGS}
        for o in self.ops:
            if o["bar"]:
                pend = list(last_sig.values()) + [(("dma", k), v) for k, v in dma_cnt.items()]
                for e in self.ENGS:
                    pending[e] = list(pend)
                continue
            e = o["eng"]
            deps = set()
            for k in o["reads"]:
                if k in last_w:
                    deps.add(last_w[k])
            for k in o["writes"]:
                if k in last_w:
                    deps.add(last_w[k])
                for r in readers.get(k, ()):
                    deps.add(r)
            deps.discard(o["idx"])
            wl = list(pending[e])
            pending[e] = []
            for d in sorted(deps):
                p = self.ops[d]
                if p["dma_key"] is None and p["eng"] == e and e == "tensor":
                    continue
                if p["dma_key"] is not None:
                    wl.append((("dma", p["dma_key"]), dma_cnt[p["dma_key"]]))
                else:
                    wl.append(p["sig"])
            for sem, val in wl:
                if waited[e].get(sem, 0) >= val:
                    continue
                waited[e][sem] = val
                o["waits"].append((sem, val))
            if o["dma_key"] is not None:
                dma_cnt[o["dma_key"]] = dma_cnt.get(o["dma_key"], 0) + 16
                o["inc"] = (("dma", o["dma_key"]), 16)
            else:
                c = eng_cnt[e]
                sem = ("eng", e, c // SEM_CAP)
                eng_cnt[e] = c + 1
                o["sig"] = (sem, c % SEM_CAP + 1)
                o["inc"] = (sem, 1)
                last_sig[e] = o["sig"]
            for k in o["reads"]:
                readers.setdefault(k, []).append(o["idx"])
            for k in o["writes"]:
                last_w[k] = o["idx"]
                readers[k] = []
        self.dma_cnt = dma_cnt
        sems = []
        seen = set()
        for o in self.ops:
            if o["bar"]:
                continue
            if o["inc"][0] not in seen:
                seen.add(o["inc"][0])
                sems.append(o["inc"][0])
        self.sem_names = sems

    def emit(self, final_wait_keys=()):
        nc = self.nc
        self.analyze()
        with ExitStack() as es:
            semobj = {}
            for i, s in enumerate(self.sem_names):
                semobj[s] = es.enter_context(nc.semaphore("s%d" % i))
            block = es.enter_context(nc.Block())
            ops = [o for o in self.ops if not o["bar"]]
            dma_cnt = self.dma_cnt

            def gen(ename):
                def body(eng):
                    for o in ops:
                        if o["eng"] != ename:
                            continue
                        for (s, v) in o["waits"]:
                            eng.wait_ge(semobj[s], v)
                        ins = o["fn"](eng)
                        s, n = o["inc"]
                        ins.then_inc(semobj[s], n)
                    if ename == "sync":
                        for k in final_wait_keys:
                            eng.wait_ge(semobj[("dma", k)], dma_cnt[k])
                return body

            block.sync(gen("sync"))
            block.scalar(gen("scalar"))
            block.vector(gen("vector"))
            block.gpsimd(gen("gpsimd"))
            block.tensor(gen("tensor"))


class Arena:
    def __init__(self, ap, ncols):
        self.ap, self.n, self.off = ap, ncols, 0

    def reset(self):
        self.off = 0

    def alloc(self, cols, dt=F32):
        w = cols if dt == F32 else (cols + 1) // 2
        a = self.ap[:, self.off:self.off + w]
        self.off += w
        assert self.off <= self.n, ("arena overflow", self.off)
        return a if dt == F32 else a.bitcast(dt)


def build(S, phases="ABCDE"):
    NO = S // 2
    NB = S // 512
    NBO = NO // 512
    NKT = S // 128
    nc = bass.Bass("TRN2", target_bir_lowering=False)

    def din(name, shape, dt=F32):
        return nc.dram_tensor(name, list(shape), dt, kind="ExternalInput").ap()

    def dscr(name, shape, dt):
        return nc.dram_tensor(name, list(shape), dt, kind="Internal").ap()

    xT = din("xT", [1024, S])
    pinfo = din("pinfo", [128, 2])
    cosT = nc.dram_tensor("cosS", [32, S], F32, kind="Internal").ap()
    sinT = nc.dram_tensor("sinS", [32, S], F32, kind="Internal").ap()
    norm_mix = din("norm_mix", [1024])
    w_in = din("w_in", [1024, 2976])
    q_a_norm = din("q_a_norm", [256])
    w_q_b = din("w_q_b", [256, 768])
    kv_a_norm = din("kv_a_norm", [128])
    w_kv_b = din("w_kv_b", [128, 1024])
    w_o_attn = din("w_o_attn", [512, 1024])
    lam_re = din("lam_re", [2, 32, 64])
    lam_im = din("lam_im", [2, 32, 64])
    log_dt = din("log_dt", [64])
    b_re = din("b_re", [2, 32, 64, 16])
    b_im = din("b_im", [2, 32, 64, 16])
    c_re = din("c_re", [2, 32, 16, 64])
    c_im = din("c_im", [2, 32, 16, 64])
    d_skip = din("d_skip", [512])
    w_glu = din("w_glu", [512, 1024])
    w_o_ssm = din("w_o_ssm", [512, 1024])
    w_out = din("w_out", [1024, 1024])
    norm_ffn = din("norm_ffn", [1024])
    w_query = din("w_query", [1024, 1024])
    sub_keys = din("sub_keys", [8, 2, 128, 64])
    w_downT = din("w_downT", [1024, 16384])
    w_up = din("w_up", [16384, 1024])
    final_norm = din("final_norm", [1024])
    ident_in = din("ident", [128, 128])
    i2_in = din("i2c", [128, 128])
    outT = nc.dram_tensor("outT", [1024, NO], F32, kind="ExternalOutput").ap()

    latT = dscr("latT", [128, S], BF16)
    krT = dscr("krT", [32, S], BF16)
    uT = dscr("uT", [512, S], BF16)
    hqT = dscr("hqT", [256, NO], BF16)
    ysT = dscr("ysT", [512, NO], BF16)
    oT = dscr("oT", [8, 64, NO], BF16)

    P = Prog(nc)
    es = ExitStack()
    with es:
        arena_t = es.enter_context(nc.sbuf_tensor("arena", [128, 45600], F32))
        consts = es.enter_context(nc.sbuf_tensor("consts", [128, 1024], F32))
        AR = Arena(arena_t, 45600)
        pbs = [es.enter_context(nc.psum_tensor("pb%d" % i, [128, 512], F32)) for i in range(8)]
        pbk = [("pb", i) for i in range(8)]

        def mm(out, lhsT, rhs, start, stop, reads, writes):
            P.op("tensor", lambda e: e.matmul(out, lhsT=lhsT, rhs=rhs, start=start, stop=stop), reads, writes)

        def tr(out, in_, ident, reads, writes):
            P.op("tensor", lambda e: e.transpose(out, in_, ident), reads, writes)

        def act(out, in_, func, reads, writes, bias=None, scale=None):
            kw = {}
            if bias is not None:
                kw["bias"] = bias
            if scale is not None:
                kw["scale"] = scale
            P.op("scalar", lambda e: e.activation(out=out, in_=in_, func=func, **kw), reads, writes)

        def tt(out, in0, in1, op, reads, writes, eng="vector"):
            P.op(eng, lambda e: e.tensor_tensor(out=out, in0=in0, in1=in1, op=op), reads, writes)

        def ts(out, in0, s1, op0, reads, writes, s2=None, op1=None, eng="vector"):
            if op1 is None:
                P.op(eng, lambda e: e.tensor_scalar(out=out, in0=in0, scalar1=s1, scalar2=None, op0=op0), reads, writes)
            else:
                P.op(eng, lambda e: e.tensor_scalar(out=out, in0=in0, scalar1=s1, scalar2=s2, op0=op0, op1=op1), reads, writes)

        def stt(out, in0, scalar, in1, op0, op1, reads, writes):
            P.op("vector", lambda e: e.scalar_tensor_tensor(out=out, in0=in0, scalar=scalar, in1=in1, op0=op0, op1=op1), reads, writes)

        def cp(out, in_, reads, writes, eng="vector"):
            if eng == "scalar":
                act(out, in_, AF.Copy, reads, writes)
            else:
                P.op(eng, lambda e: e.tensor_copy(out=out, in_=in_), reads, writes)

        def mset(ap, val, writes, eng="vector"):
            P.op(eng, lambda e: e.memset(ap, val), (), writes)

        def recip(out, in_, reads, writes):
            P.op("vector", lambda e: e.reciprocal(out=out, in_=in_), reads, writes)

        def dma(eng, out, in_, key, reads, writes, slow=False):
            if slow:
                P.dma(eng, lambda e: e.dma_start(out=out, in_=in_, allow_slow_non_contiguous=True), key, reads, writes)
            else:
                P.dma(eng, lambda e: e.dma_start(out=out, in_=in_), key, reads, writes)

        ident_f = consts[:, 0:128]
        i2c = consts[:, 128:256]
        ident_b = consts[:, 256:320].bitcast(BF16)
        ones_b = consts[:, 320:384].bitcast(BF16)
        ones_f = consts[:, 384:512]
        gmix = consts[:, 512:520]
        gffn = consts[:, 520:528]
        gfin = consts[:, 528:536]
        gq = consts[:, 536:538]
        gkv = consts[:, 538:539]
        dsk = consts[:, 540:544]
        epsc = consts[:, 544:545]
        dma("sync", ident_f, ident_in, "c_id", (), ["ident_f"])
        dma("sync", i2c, i2_in, "c_i2", (), ["i2c"])
        cp(ident_b, ident_f, ["ident_f"], ["ident_b"])
        mset(ones_b, 1.0, ["ones_b"])
        mset(ones_f, 1.0, ["ones_f"])
        mset(epsc, EPS, ["epsc"])
        for nm, dst, src, k in (("gmix", gmix, norm_mix, 8), ("gffn", gffn, norm_ffn, 8), ("gfin", gfin, final_norm, 8),
                                ("gq", gq, q_a_norm, 2), ("gkv", gkv, kv_a_norm, 1), ("dsk", dsk, d_skip, 4)):
            dma("sync", dst, src.rearrange("(k p) -> p k", p=128), "c_" + nm, (), [nm], slow=True)

        rr = {"ps": 0}

        def psb(n=1):
            i = rr["ps"] % 8
            rr["ps"] += 1
            return pbs[i], pbk[i]

        def norm_rstd(sq_ap_list, nfeat, out_bc, reads, okey, pre=None):
            pb, pk = psb()
            n = len(sq_ap_list)
            for i, a in enumerate(sq_ap_list):
                mm(pb[:, :], ones_b[0:a.shape[0], :], a, i == 0, i == n - 1, reads + ["ones_b"], [pk])
            if pre is None:
                act(out_bc, pb[:, :], AF.Sqrt, [pk, "epsc"], [okey], bias=epsc[:, 0:1], scale=1.0 / nfeat)
            else:
                tt(out_bc, pb[:, :], pre[0], ALU.mult, [pk, pre[1]], [okey])
                tt(out_bc, out_bc, pre[0], ALU.mult, [okey, pre[1]], [okey])
                act(out_bc, out_bc, AF.Sqrt, [okey, "epsc"], [okey], bias=epsc[:, 0:1], scale=1.0 / nfeat)
            recip(out_bc, out_bc, [okey], [okey])

        def load_scaled_w(dst_b, src, kt_n, ncols, gtile, gkey, key, stage, skey, c0=0):
            for kt in range(kt_n):
                for c in range(0, ncols, 1024):
                    w = min(1024, ncols - c)
                    dma("sync", stage[:, 0:w], src[kt * 128:(kt + 1) * 128, c0 + c:c0 + c + w], "d_" + skey, (), [skey])
                    if gtile is None:
                        cp(dst_b[:, kt, c:c + w], stage[:, 0:w], [skey], [key])
                    else:
                        ts(dst_b[:, kt, c:c + w], stage[:, 0:w], gtile[:, kt:kt + 1], ALU.mult, [skey, gkey], [key])

        def x_block(blk, xf, xb, sq, rstd, tag):
            dma("sync", xf, xT[:, blk * 512:(blk + 1) * 512].rearrange("(k p) t -> p k t", p=128), "d_xf" + tag, (), ["xf" + tag])
            cp(xb, xf, ["xf" + tag], ["xb" + tag], eng="gpsimd")
            act(sq, xf, AF.Square, ["xf" + tag], ["sq" + tag])
            norm_rstd([sq[:, k, :] for k in range(8)], 1024.0, rstd, ["sq" + tag], "rstd" + tag)

        if "A" in phases or "C" in phases:
            I32 = mybir.dt.int32
            CW = S // 4
            p0_base = AR.n - (5 * CW + 16)
            AR.off = p0_base
            pin = AR.alloc(2)
            dma("sync", pin, pinfo, "d_pin", (), ["pin"])
            ji = AR.alloc(1).bitcast(I32)
            jf = AR.alloc(1); jm = AR.alloc(1); ifr = AR.alloc(1)
            for g in range(4):
                P.op("gpsimd", (lambda g: lambda e: e.iota(ji[g * 32:(g + 1) * 32, :], pattern=[[1, 1]], base=0, channel_multiplier=1))(g), (), ["ji"])
            cp(jf, ji, ["ji"], ["jf"])
            ts(jm, jf, 16.0, ALU.is_ge, ["jf"], ["jm"], s2=-16.0, op1=ALU.mult)
            tt(jf, jf, jm, ALU.add, ["jf", "jm"], ["jf"])
            act(ifr, jf, AF.Exp, ["jf"], ["ifr"], scale=float(-np.log(10000.0) / 16.0))
            tbuf = AR.alloc(CW); ang = AR.alloc(CW); kq = AR.alloc(CW); rr_ = AR.alloc(CW); wm_ = AR.alloc(CW)
            ti = tbuf.bitcast(I32); kqi = tbuf.bitcast(I32)
            so = ang; r2 = kq; co = rr_

            def wrap0(o, okey):
                ts(wm_, o, float(-np.pi), ALU.is_lt, [okey], ["wm_"], s2=float(2 * np.pi), op1=ALU.mult)
                tt(o, o, wm_, ALU.add, [okey, "wm_"], [okey])
                ts(wm_, o, float(np.pi), ALU.is_gt, [okey], ["wm_"], s2=float(-2 * np.pi), op1=ALU.mult)
                tt(o, o, wm_, ALU.add, [okey, "wm_"], [okey])

            for g in range(4):
                P.op("gpsimd", (lambda g: lambda e: e.iota(ti[g * 32:(g + 1) * 32, :], pattern=[[1, CW]], base=g * CW, channel_multiplier=0))(g), (), ["tbuf"])
            cp(ang, ti, ["tbuf"], ["ang"])
            ts(ang, ang, pin[:, 0:1], ALU.mult, ["ang", "pin"], ["ang"], s2=pin[:, 1:2], op1=ALU.add)
            ts(ang, ang, ifr[:, 0:1], ALU.mult, ["ang", "ifr"], ["ang"])
            ts(kq, ang, float(1.0 / (2 * np.pi)), ALU.mult, ["ang"], ["kq"], s2=0.5, op1=ALU.add)
            cp(kqi, kq, ["kq"], ["tbuf"])
            cp(kq, kqi, ["tbuf"], ["kq"])
            stt(rr_, kq, -6.28125, ang, ALU.mult, ALU.add, ["kq", "ang"], ["rr_"])
            stt(rr_, kq, float(-(2 * np.pi - 6.28125)), rr_, ALU.mult, ALU.add, ["kq", "rr_"], ["rr_"])
            wrap0(rr_, "rr_")
            act(so, rr_, AF.Sin, ["rr_"], ["ang"])
            ts(r2, rr_, float(np.pi / 2), ALU.add, ["rr_"], ["kq"])
            wrap0(r2, "kq")
            act(co, r2, AF.Sin, ["kq"], ["rr_"])
            for g in range(4):
                dma("sync", sinT[:, g * CW:(g + 1) * CW], so[g * 32:(g + 1) * 32, :], "st_so", ["ang"], ["ropeS"])
                dma("scalar", cosT[:, g * CW:(g + 1) * CW], co[g * 32:(g + 1) * 32, :], "st_co", ["rr_"], ["ropeC"])
            P0_BASE = p0_base

        if "A" in phases:
            AR.reset()
            stage = AR.alloc(1024)
            Wa = AR.alloc(8 * 928, BF16).rearrange("p (k c) -> p k c", k=8)
            Wkr = AR.alloc(8 * 96, BF16).rearrange("p (k c) -> p k c", k=8)
            Wkrr = AR.alloc(8 * 96, BF16).rearrange("p (k c) -> p k c", k=8)
            load_scaled_w(Wa, w_in, 8, 928, gmix, "gmix", "Wa", stage, "stage")
            mset(Wkr, 0.0, ["Wkr"])
            mset(Wkrr, 0.0, ["Wkrr"])
            cp(Wkr[:, :, 64:96], Wa[:, :, 384:416], ["Wa"], ["Wkr"])
            ts(Wkrr[:, :, 64:80], Wa[:, :, 400:416], -1.0, ALU.mult, ["Wa"], ["Wkrr"])
            cp(Wkrr[:, :, 80:96], Wa[:, :, 384:400], ["Wa"], ["Wkrr"])
            xfs = [AR.alloc(8 * 512).rearrange("p (k t) -> p k t", k=8) for _ in range(2)]
            xbs = [AR.alloc(8 * 512, BF16).rearrange("p (k t) -> p k t", k=8) for _ in range(2)]
            sqs = [AR.alloc(8 * 512, BF16).rearrange("p (k t) -> p k t", k=8) for _ in range(2)]
            rstds = [AR.alloc(512) for _ in range(2)]
            cst = [AR.alloc(512) for _ in range(2)]
            snt = [AR.alloc(512) for _ in range(2)]
            tmpf = [AR.alloc(1024) for _ in range(2)]
            tmpb = [AR.alloc(1024, BF16) for _ in range(2)]
            r2 = [AR.alloc(512) for _ in range(2)]
            outb = [AR.alloc(512 * 8, BF16) for _ in range(2)]
            assert AR.off <= P0_BASE, (AR.off, P0_BASE)
            for blk in range(NB):
                s = blk % 2
                tg = "A%d" % s
                xf, xb, sq, rstd = xfs[s], xbs[s], sqs[s], rstds[s]
                x_block(blk, xf, xb, sq, rstd, tg)
                ob = outb[s]
                okey = "outb" + tg
                dma("scalar", cst[s][64:96, :], cosT[:, blk * 512:(blk + 1) * 512], "d_cs" + tg, ["ropeC"], ["cs" + tg])
                dma("scalar", snt[s][64:96, :], sinT[:, blk * 512:(blk + 1) * 512], "d_sn" + tg, ["ropeS"], ["sn" + tg])
                pb, pk = psb()
                for k in range(8):
                    mm(pb[:, :], Wa[:, k, 256:384], xb[:, k, :], k == 0, k == 7, ["Wa", "xb" + tg], [pk])
                tf = tmpf[s]
                tt(tf[:, 0:512], pb[:, :], rstd, ALU.mult, [pk, "rstd" + tg], ["tf" + tg])
                act(tmpb[s][:, 0:512], tf[:, 0:512], AF.Square, ["tf" + tg], ["tb" + tg])
                norm_rstd([tmpb[s][:, 0:512]], 128.0, r2[s], ["tb" + tg], "r2" + tg)
                stt(ob[:, 0:512], tf[:, 0:512], gkv[:, 0:1], r2[s], ALU.mult, ALU.mult, ["tf" + tg, "gkv", "r2" + tg], [okey])
                dma("sync", latT[:, blk * 512:(blk + 1) * 512], ob[:, 0:512], "st_" + okey, [okey], ["latT"])
                pa, pka = psb()
                pb2, pkb = psb()
                for k in range(8):
                    mm(pa[0:96, :], Wkr[:, k, :], xb[:, k, :], k == 0, k == 7, ["Wkr", "xb" + tg], [pka])
                for k in range(8):
                    mm(pb2[0:96, :], Wkrr[:, k, :], xb[:, k, :], k == 0, k == 7, ["Wkrr", "xb" + tg], [pkb])
                tt(tf[64:96, 0:512], pa[64:96, :], cst[s][64:96, :], ALU.mult, [pka, "cs" + tg], ["tf" + tg])
                tt(tf[64:96, 512:1024], pb2[64:96, :], snt[s][64:96, :], ALU.mult, [pkb, "sn" + tg], ["tf" + tg])
                tt(tf[64:96, 0:512], tf[64:96, 0:512], tf[64:96, 512:1024], ALU.add, ["tf" + tg], ["tf" + tg])
                tt(ob[64:96, 512:1024], tf[64:96, 0:512], rstd[64:96, :], ALU.mult, ["tf" + tg, "rstd" + tg], [okey])
                dma("sync", krT[:, blk * 512:(blk + 1) * 512], ob[64:96, 512:1024], "st_" + okey, [okey], ["krT"])
                for j in range(4):
                    pb, pk = psb()
                    for k in range(8):
                        mm(pb[:, :], Wa[:, k, 416 + j * 128:416 + (j + 1) * 128], xb[:, k, :], k == 0, k == 7, ["Wa", "xb" + tg], [pk])
                    tt(ob[:, 1024 + j * 512:1024 + (j + 1) * 512], pb[:, :], rstd, ALU.mult, [pk, "rstd" + tg], [okey])
                dma("sync", uT[:, blk * 512:(blk + 1) * 512].rearrange("(j p) t -> p j t", p=128),
                    ob[:, 1024:3072].rearrange("p (j t) -> p j t", j=4), "st_" + okey, [okey], ["uT"])
                if blk < NBO:
                    for j in range(2):
                        pb, pk = psb()
                        for k in range(8):
                            mm(pb[:, :], Wa[:, k, j * 128:(j + 1) * 128], xb[:, k, :], k == 0, k == 7, ["Wa", "xb" + tg], [pk])
                        tt(tf[:, j * 512:(j + 1) * 512], pb[:, :], rstd, ALU.mult, [pk, "rstd" + tg], ["tf" + tg])
                    act(tmpb[s][:, 0:1024], tf[:, 0:1024], AF.Square, ["tf" + tg], ["tb" + tg])
                    norm_rstd([tmpb[s][:, 0:512], tmpb[s][:, 512:1024]], 256.0, r2[s], ["tb" + tg], "r2" + tg)
                    for j in range(2):
                        stt(ob[:, 3072 + j * 512:3072 + (j + 1) * 512], tf[:, j * 512:(j + 1) * 512], gq[:, j:j + 1], r2[s],
                            ALU.mult, ALU.mult, ["tf" + tg, "gq", "r2" + tg], [okey])
                    dma("sync", hqT[:, blk * 512:(blk + 1) * 512].rearrange("(j p) t -> p j t", p=128),
                        ob[:, 3072:4096].rearrange("p (j t) -> p j t", j=2), "st_" + okey, [okey], ["hqT"])
            P.barrier()

        if "B" in phases:
            AR.reset()
            nsteps_b = S.bit_length() - 1
            nsteps_f = NO.bit_length() - 1
            lre = AR.alloc(64); lim = AR.alloc(64); dtb = AR.alloc(64)
            for hlf in range(2):
                dma("sync", lre[hlf * 64:(hlf + 1) * 64, :], lam_re.rearrange("d g p -> p (d g)"), "d_lre", (), ["lre"], slow=True)
                dma("sync", lim[hlf * 64:(hlf + 1) * 64, :], lam_im.rearrange("d g p -> p (d g)"), "d_lim", (), ["lim"], slow=True)
            dma("sync", dtb, log_dt.partition_broadcast(128), "d_dt", (), ["dtb"])
            act(dtb, dtb, AF.Exp, ["dtb"], ["dtb"])
            al = AR.alloc(64); th = AR.alloc(64); kk = AR.alloc(64); ki = AR.alloc(64).bitcast(mybir.dt.int32)
            tt(al, lre, dtb, ALU.mult, ["lre", "dtb"], ["al"])
            tt(th, lim, dtb, ALU.mult, ["lim", "dtb"], ["th"])
            rmag = AR.alloc(64)
            act(rmag, al, AF.Exp, ["al"], ["rmag"])
            ts(kk, th, 1.0 / (2 * np.pi), ALU.mult, ["th"], ["kk"], s2=0.5, op1=ALU.add)
            cp(ki, kk, ["kk"], ["ki"])
            cp(kk, ki, ["ki"], ["kk"])
            thr = AR.alloc(64)
            stt(thr, kk, -2 * np.pi, th, ALU.mult, ALU.add, ["kk", "th"], ["thr"])
            sn = AR.alloc(64); cs = AR.alloc(64); wr = AR.alloc(64)
            wm = AR.alloc(64)

            def wrap(o, i, shift, okey):
                ts(o, i, float(shift), ALU.add, ["thr"], [okey])
                ts(wm, o, float(-np.pi), ALU.is_lt, [okey], ["wm"], s2=float(2 * np.pi), op1=ALU.mult)
                tt(o, o, wm, ALU.add, [okey, "wm"], [okey])
                ts(wm, o, float(np.pi), ALU.is_gt, [okey], ["wm"], s2=float(-2 * np.pi), op1=ALU.mult)
                tt(o, o, wm, ALU.add, [okey, "wm"], [okey])
            wrap(wr, thr, 0.0, "wr")
            act(sn, wr, AF.Sin, ["wr"], ["sn"])
            wr2 = AR.alloc(64)
            wrap(wr2, thr, np.pi / 2, "wr2")
            act(cs, wr2, AF.Sin, ["wr2"], ["cs"])
            are = AR.alloc(64); aim = AR.alloc(64)
            tt(are, rmag, cs, ALU.mult, ["rmag", "cs"], ["are"])
            tt(aim, rmag, sn, ALU.mult, ["rmag", "sn"], ["aim"])
            den = AR.alloc(64); t1 = AR.alloc(64); t2 = AR.alloc(64); am1 = AR.alloc(64); kre = AR.alloc(64); kim = AR.alloc(64)
            tt(den, lre, lre, ALU.mult, ["lre"], ["den"])
            tt(t1, lim, lim, ALU.mult, ["lim"], ["t1"])
            tt(den, den, t1, ALU.add, ["den", "t1"], ["den"])
            recip(den, den, ["den"], ["den"])
            ts(am1, are, -1.0, ALU.add, ["are"], ["am1"])
            tt(t1, am1, lre, ALU.mult, ["am1", "lre"], ["t1"])
            tt(t2, aim, lim, ALU.mult, ["aim", "lim"], ["t2"])
            tt(kre, t1, t2, ALU.add, ["t1", "t2"], ["kre"])
            tt(kre, kre, den, ALU.mult, ["kre", "den"], ["kre"])
            tt(t1, aim, lre, ALU.mult, ["aim", "lre"], ["t1"])
            tt(t2, am1, lim, ALU.mult, ["am1", "lim"], ["t2"])
            tt(kim, t1, t2, ALU.subtract, ["t1", "t2"], ["kim"])
            tt(kim, kim, den, ALU.mult, ["kim", "den"], ["kim"])
            import os
            BSTOP = int(os.environ.get("BSTOP", "99"))
            Bz = AR.alloc(64 * 128, BF16).rearrange("p (g c) -> p g c", g=64)
            Cz = AR.alloc(64 * 128, BF16).rearrange("p (g c) -> p g c", g=64)
            pw_re = AR.alloc(nsteps_b * 64).rearrange("p (k g) -> p k g", k=nsteps_b)
            pw_im = AR.alloc(nsteps_b * 64).rearrange("p (k g) -> p k g", k=nsteps_b)
            NM = 3 * ((nsteps_b) // 2) + (nsteps_b % 2)
            c0 = AR.alloc(NM * 64).rearrange("p (k g) -> p k g", k=NM)
            c1 = AR.alloc(NM * 64).rearrange("p (k g) -> p k g", k=NM)
            PR = AR.alloc(NM * 64).rearrange("p (k g) -> p k g", k=NM)
            PI = AR.alloc(NM * 64).rearrange("p (k g) -> p k g", k=NM)
            mark = AR.off
            bre = AR.alloc(1024).rearrange("p (g h) -> p g h", g=64)
            bim = AR.alloc(1024).rearrange("p (g h) -> p g h", g=64)
            for hlf in range(2):
                dma("sync", bre[hlf * 64:(hlf + 1) * 64], b_re.rearrange("d g p h -> p (d g) h"), "d_bre", (), ["bre"])
                dma("sync", bim[hlf * 64:(hlf + 1) * 64], b_im.rearrange("d g p h -> p (d g) h"), "d_bim", (), ["bim"])
            bb = AR.alloc(1024).rearrange("p (g h) -> p g h", g=64)
            tb1 = AR.alloc(1024).rearrange("p (g h) -> p g h", g=64)
            kre_b = kre.unsqueeze(2).to_broadcast([128, 64, 16])
            kim_b = kim.unsqueeze(2).to_broadcast([128, 64, 16])
            tt(bb[0:64], bre[0:64], kre_b[0:64], ALU.mult, ["bre", "kre"], ["bb"])
            tt(tb1[0:64], bim[0:64], kim_b[0:64], ALU.mult, ["bim", "kim"], ["tb1"])
            tt(bb[0:64], bb[0:64], tb1[0:64], ALU.subtract, ["bb", "tb1"], ["bb"])
            tt(bb[64:128], bim[64:128], kre_b[64:128], ALU.mult, ["bim", "kre"], ["bb"])
            tt(tb1[64:128], bre[64:128], kim_b[64:128], ALU.mult, ["bre", "kim"], ["tb1"])
            tt(bb[64:128], bb[64:128], tb1[64:128], ALU.add, ["bb", "tb1"], ["bb"])
            Sg = AR.alloc(128)
            for gd in range(64):
                sl = gd % 8
                mset(Sg, 0.0, ["Sg"])
                cp(Sg[:, sl * 16:(sl + 1) * 16], bb[:, gd, :], ["bb"], ["Sg"])
                pb, pk = psb()
                tr(pb[:, 0:128], Sg, ident_f, ["Sg", "ident_f"], [pk])
                cp(Bz[:, gd, :], pb[:, 0:128], [pk], ["Bz"], eng="scalar")
            cst_ = AR.alloc(64 * 128, BF16).rearrange("p (g c) -> p g c", g=64)
            mset(cst_, 0.0, ["cst_"])
            for sl in range(8):
                dma("gpsimd", cst_[sl * 16:(sl + 1) * 16].rearrange("p (d q s) c -> p d q s c", d=2, q=4, s=8)[:, :, :, sl, 0:64],
                    c_re.rearrange("d (q s) h p -> s h d q p", s=8)[sl], "d_cst", (), ["cst_"])
                dma("gpsimd", cst_[sl * 16:(sl + 1) * 16].rearrange("p (d q s) c -> p d q s c", d=2, q=4, s=8)[:, :, :, sl, 64:128],
                    c_im.rearrange("d (q s) h p -> s h d q p", s=8)[sl], "d_cst", (), ["cst_"])
            ts(cst_[:, :, 64:128], cst_[:, :, 64:128], -1.0, ALU.mult, ["cst_"], ["cst_"])
            for gd in range(64):
                pb, pk = psb()
                pbb = pb[:, 0:64].bitcast(BF16)
                tr(pbb, cst_[:, gd, :], ident_b, ["cst_", "ident_b"], [pk])
                cp(Cz[:, gd, :], pbb, [pk], ["Cz"], eng="scalar")
            cp(pw_re[:, 0, :], are, ["are"], ["pw"])
            cp(pw_im[:, 0, :], aim, ["aim"], ["pw"])
            for k in range(1, nsteps_b):
                tt(t1, pw_re[:, k - 1, :], pw_re[:, k - 1, :], ALU.mult, ["pw"], ["t1"])
                tt(t2, pw_im[:, k - 1, :], pw_im[:, k - 1, :], ALU.mult, ["pw"], ["t2"])
                tt(pw_re[:, k, :], t1, t2, ALU.subtract, ["t1", "t2"], ["pw"])
                tt(t1, pw_re[:, k - 1, :], pw_im[:, k - 1, :], ALU.mult, ["pw"], ["t1"])
                ts(pw_im[:, k, :], t1, 2.0, ALU.mult, ["t1"], ["pw"])
            steps_all = []
            w_ = 1
            while w_ < S:
                if w_ * 4 <= S:
                    steps_all.append((w_, [1, 2, 3])); w_ *= 4
                else:
                    steps_all.append((w_, [1])); w_ *= 2
            midx = {}
            for (sh_, ms_) in steps_all:
                for m_ in ms_:
                    midx[(sh_, m_)] = len(midx)
            for (sh_, m_), ix in midx.items():
                k_ = sh_.bit_length() - 1
                if m_ == 1:
                    cp(PR[:, ix, :], pw_re[:, k_, :], ["pw"], ["PRI"])
                    cp(PI[:, ix, :], pw_im[:, k_, :], ["pw"], ["PRI"])
                elif m_ == 2:
                    cp(PR[:, ix, :], pw_re[:, k_ + 1, :], ["pw"], ["PRI"])
                    cp(PI[:, ix, :], pw_im[:, k_ + 1, :], ["pw"], ["PRI"])
                else:
                    tt(t1, pw_re[:, k_, :], pw_re[:, k_ + 1, :], ALU.mult, ["pw"], ["t1"])
                    tt(t2, pw_im[:, k_, :], pw_im[:, k_ + 1, :], ALU.mult, ["pw"], ["t2"])
                    tt(PR[:, ix, :], t1, t2, ALU.subtract, ["t1", "t2"], ["PRI"])
                    tt(t1, pw_re[:, k_, :], pw_im[:, k_ + 1, :], ALU.mult, ["pw"], ["t1"])
                    tt(t2, pw_im[:, k_, :], pw_re[:, k_ + 1, :], ALU.mult, ["pw"], ["t2"])
                    tt(PI[:, ix, :], t1, t2, ALU.add, ["t1", "t2"], ["PRI"])
            cp(c0[0:64], PR[0:64], ["PRI"], ["c01"])
            ts(c0[64:128], PI[64:128], -1.0, ALU.mult, ["PRI"], ["c01"])
            cp(c1[0:64], PI[0:64], ["PRI"], ["c01"])
            cp(c1[64:128], PR[64:128], ["PRI"], ["c01"])
            if BSTOP <= 1:
                j_range = []
            else:
                j_range = range(4)
            P.barrier()
            AR.off = mark
            ut = AR.alloc(S, BF16)
            Xf = AR.alloc(S)
            Xb = AR.alloc(S, BF16)
            ysum = AR.alloc(NO)
            ysb = AR.alloc(NO, BF16)
            Mk = [AR.alloc(NM * 128, BF16).rearrange("p (k c) -> p k c", k=NM) for _ in range(2)]
            gi = 0
            for j in j_range:
                dma("sync", ut, uT[j * 128:(j + 1) * 128, :], "d_ut", (), ["ut"])
                first = True
                for g in range(j * 8, j * 8 + 8):
                    for d in range(2):
                        gd = d * 32 + g
                        ncol = NO if d == 0 else S
                        steps = []
                        for (sh_, ms_) in steps_all:
                            if 4 * sh_ <= ncol:
                                steps.append((sh_, [m_ for m_ in ms_]))
                            elif 2 * sh_ <= ncol:
                                steps.append((sh_, [1]))
                        nblk = ncol // 512
                        mk = Mk[gi % 2]
                        mkey = "Mk%d" % (gi % 2)
                        gi += 1
                        for (sh_, ms_) in steps:
                            for m_ in ms_:
                                ix = midx[(sh_, m_)]
                                ts(mk[:, ix, 0:64], i2c[:, 0:64], c0[:, ix, gd:gd + 1], ALU.mult, ["i2c", "c01"], [mkey], eng="gpsimd")
                                ts(mk[:, ix, 64:128], i2c[:, 64:128], c1[:, ix, gd:gd + 1], ALU.mult, ["i2c", "c01"], [mkey], eng="gpsimd")
                        for b in range(nblk):
                            pb, pk = psb()
                            mm(pb[:, :], Bz[:, gd, :], ut[:, b * 512:(b + 1) * 512], True, True, ["Bz", "ut"], [pk])
                            cp(Xf[:, b * 512:(b + 1) * 512], pb[:, :], [pk], [("Xf", b)], eng="scalar")
                            cp(Xb[:, b * 512:(b + 1) * 512], pb[:, :], [pk], [("Xb", b)])
                        for (sh_, ms_) in steps:
                            order = range(nblk - 1, -1, -1) if d == 0 else range(nblk)
                            for b in order:
                                rngs = []
                                for m_ in ms_:
                                    off = m_ * sh_
                                    if d == 0:
                                        t0 = max(b * 512, off); t1_ = (b + 1) * 512
                                        s0 = t0 - off
                                    else:
                                        t0 = b * 512; t1_ = min((b + 1) * 512, ncol - off)
                                        s0 = t0 + off
                                    if t0 < t1_:
                                        rngs.append((m_, t0, t1_, s0))
                                if not rngs:
                                    continue
                                _, T0, T1_, _ = rngs[0]
                                pb, pk = psb()
                                for ri, (m_, t0, t1_, s0) in enumerate(rngs):
                                    n = t1_ - t0
                                    sblks = sorted(set([s0 // 512, (s0 + n - 1) // 512]))
                                    mm(pb[:, t0 - T0:t0 - T0 + n], mk[:, midx[(sh_, m_)], :], Xb[:, s0:s0 + n], ri == 0, ri == len(rngs) - 1,
                                       [mkey] + [("Xb", q) for q in sblks], [pk])
                                N = T1_ - T0
                                tt(Xf[:, T0:T1_], Xf[:, T0:T1_], pb[:, 0:N], ALU.add, [pk, ("Xf", b)], [("Xf", b)])
                                cp(Xb[:, T0:T1_], Xf[:, T0:T1_], [("Xf", b)], [("Xb", b)], eng="scalar")
                        for b in range(NBO):
                            pb, pk = psb()
                            mm(pb[:, :], Cz[:, gd, :], Xb[:, b * 512:(b + 1) * 512], True, True, ["Cz", ("Xb", b)], [pk])
                            if first:
                                cp(ysum[:, b * 512:(b + 1) * 512], pb[:, :], [pk], [("ys", b)])
                            else:
                                tt(ysum[:, b * 512:(b + 1) * 512], ysum[:, b * 512:(b + 1) * 512], pb[:, :], ALU.add, [pk, ("ys", b)], [("ys", b)])
                        first = False
                for b in range(NBO):
                    stt(ysum[:, b * 512:(b + 1) * 512], ut[:, b * 512:(b + 1) * 512], dsk[:, j:j + 1], ysum[:, b * 512:(b + 1) * 512],
                        ALU.mult, ALU.add, ["ut", "dsk", ("ys", b)], [("ys", b)])
                    act(ysb[:, b * 512:(b + 1) * 512], ysum[:, b * 512:(b + 1) * 512], AF.Gelu_apprx_tanh, [("ys", b)], [("ysb", b)])
                dma("sync", ysT[j * 128:(j + 1) * 128, :], ysb, "st_ysb", [("ysb", b) for b in range(NBO)], ["ysT"])
            P.barrier()

        if "C" in phases:
            AR.reset()
            scale = 96.0 ** -0.5
            stage = AR.alloc(1024)
            Wq = AR.alloc(2 * 768, BF16).rearrange("p (k c) -> p k c", k=2)
            Wqr = AR.alloc(2 * 768, BF16).rearrange("p (k c) -> p k c", k=2)
            Wkv = AR.alloc(1024, BF16).rearrange("p (k c) -> p k c", k=1)
            load_scaled_w(Wq, w_q_b, 2, 768, None, None, "Wq", stage, "stage")
            load_scaled_w(Wkv, w_kv_b, 1, 1024, None, None, "Wkv", stage, "stage")
            mset(Wqr, 0.0, ["Wqr"])
            Wq4 = Wq.rearrange("p k (h c) -> p k h c", h=8)
            Wqr4 = Wqr.rearrange("p k (h c) -> p k h c", h=8)
            for k in range(2):
                ts(Wqr4[:, k, :, 64:80], Wq4[:, k, :, 80:96], -1.0, ALU.mult, ["Wq"], ["Wqr"])
                cp(Wqr4[:, k, :, 80:96], Wq4[:, k, :, 64:80], ["Wq"], ["Wqr"])
            lat = AR.alloc(S, BF16)
            hq = AR.alloc(2 * NO, BF16).rearrange("p (k t) -> p k t", k=2)
            dma("sync", lat, latT, "d_lat", ["latT"], ["lat"])
            dma("sync", hq, hqT.rearrange("(k p) t -> p k t", p=128), "d_hq", ["hqT"], ["hq"])
            cq = AR.alloc(NO); sq_ = AR.alloc(NO)
            dma("scalar", cq[64:96, :], cosT[:, 0:NO], "d_cq", ["ropeC"], ["cq"])
            dma("scalar", sq_[64:96, :], sinT[:, 0:NO], "d_sq", ["ropeS"], ["sq_"])
            KT = [AR.alloc(S, BF16) for _ in range(2)]
            for s in range(2):
                dma("sync", KT[s][64:96, :], krT, "d_KT%d" % s, ["krT"], [("KT", s)])
                mset(KT[s][96:97, :], 1.0, [("KT", s)])
            V = [AR.alloc(NKT * 65, BF16).rearrange("p (k c) -> p k c", k=NKT) for _ in range(2)]
            for s in range(2):
                mset(V[s][:, :, 64:65], 1.0, [("V", s)])
            QT = [AR.alloc(NO, BF16) for _ in range(2)]
            sqk = [AR.alloc(512, BF16) for _ in range(2)]
            m16 = AR.alloc(max(NB, 8)); kmx = AR.alloc(1); nkm = AR.alloc(1)
            t1f = [AR.alloc(512) for _ in range(2)]
            t2f = [AR.alloc(512) for _ in range(2)]
            Pt = [AR.alloc(512, BF16) for _ in range(4)]
            osb = [AR.alloc(512) for _ in range(2)]
            obf = [AR.alloc(512, BF16) for _ in range(2)]
            pi = 0
            for h in range(8):
                s = h % 2
                ktk, vk, qk = ("KT", s), ("V", s), ("QT", s)
                for b in range(NB):
                    pb, pk = psb()
                    mm(pb[0:64, :], Wkv[:, 0, h * 128:h * 128 + 64], lat[:, b * 512:(b + 1) * 512], True, True, ["Wkv", "lat"], [pk])
                    cp(KT[s][0:64, b * 512:(b + 1) * 512], pb[0:64, :], [pk], [ktk], eng="scalar")
                    act(sqk[b % 2][0:96, :], KT[s][0:96, b * 512:(b + 1) * 512], AF.Square, [ktk], [("sqk", b % 2)])
                    pb2, pk2 = psb()
                    mm(pb2[:, :], ones_b[0:96, :], sqk[b % 2][0:96, :], True, True, [("sqk", b % 2), "ones_b"], [pk2])
                    P.op("vector", (lambda pb2, b: lambda e: e.tensor_reduce(out=m16[:, b:b + 1], in_=pb2[:, :], axis=AX.X, op=ALU.max))(pb2, b), [pk2], ["m16"])
                P.op("vector", lambda e: e.tensor_reduce(out=kmx, in_=m16[:, 0:NB], axis=AX.X, op=ALU.max), ["m16"], ["kmx"])
                act(kmx, kmx, AF.Sqrt, ["kmx"], ["kmx"])
                ts(nkm, kmx, -1.0, ALU.mult, ["kmx"], ["nkm"])
                for k0 in range(0, NKT, 8):
                    pb, pk = psb()
                    for kk_ in range(8):
                        kt = k0 + kk_
                        mm(pb[:, kk_ * 64:(kk_ + 1) * 64], lat[:, kt * 128:(kt + 1) * 128], Wkv[:, 0, h * 128 + 64:h * 128 + 128], True, True, ["Wkv", "lat"], [pk])
                    cp(V[s][:, k0:k0 + 8, 0:64], pb[:, :].rearrange("p (k c) -> p k c", k=8), [pk], [vk])
                for qb in range(NBO):
                    pa, pka = psb()
                    pr, pkr = psb()
                    for k in range(2):
                        mm(pa[0:96, :], Wq[:, k, h * 96:(h + 1) * 96], hq[:, k, qb * 512:(qb + 1) * 512], k == 0, k == 1, ["Wq", "hq"], [pka])
                    for k in range(2):
                        mm(pr[0:96, :], Wqr[:, k, h * 96:(h + 1) * 96], hq[:, k, qb * 512:(qb + 1) * 512], k == 0, k == 1, ["Wqr", "hq"], [pkr])
                    cp(QT[s][0:64, qb * 512:(qb + 1) * 512], pa[0:64, :], [pka], [qk], eng="scalar")
                    a1, a2 = t1f[qb % 2], t2f[qb % 2]
                    tt(a1[64:96, :], pa[64:96, :], cq[64:96, qb * 512:(qb + 1) * 512], ALU.mult, [pka, "cq"], [("t1f", qb % 2)])
                    tt(a2[64:96, :], pr[64:96, :], sq_[64:96, qb * 512:(qb + 1) * 512], ALU.mult, [pkr, "sq_"], [("t2f", qb % 2)])
                    tt(QT[s][64:96, qb * 512:(qb + 1) * 512], a1[64:96, :], a2[64:96, :], ALU.add, [("t1f", qb % 2), ("t2f", qb % 2)], [qk])
                    act(sqk[qb % 2][0:96, :], QT[s][0:96, qb * 512:(qb + 1) * 512], AF.Square, [qk], [("sqk", qb % 2)])
                    pn, pkn = psb()
                    mm(pn[:, :], ones_b[0:96, :], sqk[qb % 2][0:96, :], True, True, [("sqk", qb % 2), "ones_b"], [pkn])
                    act(a1[96:97, :], pn[96:97, :], AF.Sqrt, [pkn], [("t1f", qb % 2)])
                    ts(QT[s][96:97, qb * 512:(qb + 1) * 512], a1[96:97, :], nkm[96:97, 0:1], ALU.mult, [("t1f", qb % 2), "nkm"], [qk])
                for qb in range(NBO):
                    po, pko = psb()
                    prev = []
                    for kt in range(NKT):
                        pS, pkS = psb()
                        if pkS == pko:
                            pS, pkS = psb()
                        mm(pS[:, :], KT[s][0:97, kt * 128:(kt + 1) * 128], QT[s][0:97, qb * 512:(qb + 1) * 512], True, True, [ktk, qk], [pkS])
                        pt = Pt[pi % 4]; ptk = ("Pt", pi % 4); pi += 1
                        act(pt, pS[:, :], AF.Exp, [pkS], [ptk], scale=scale)
                        prev.append((kt, pt, ptk))
                        if len(prev) > 2:
                            k_, p_, pk_ = prev.pop(0)
                            mm(po[0:65, :], V[s][:, k_, 0:65], p_, k_ == 0, k_ == NKT - 1, [vk, pk_], [pko])
                    for (k_, p_, pk_) in prev:
                        mm(po[0:65, :], V[s][:, k_, 0:65], p_, k_ == 0, k_ == NKT - 1, [vk, pk_], [pko])
                    o = osb[qb % 2]; ok = ("osb", qb % 2)
                    cp(o[0:65, :], po[0:65, :], [pko], [ok])
                    recip(o[64:65, :], o[64:65, :], [ok], [ok])
                    pr, pkr = psb()
                    mm(pr[0:64, :], ones_f[64:65, 0:64], o[64:65, :], True, True, [ok, "ones_f"], [pkr])
                    ob_ = obf[qb % 2]; obk = ("obf", qb % 2)
                    tt(ob_[0:64, :], o[0:64, :], pr[0:64, :], ALU.mult, [ok, pkr], [obk])
                    dma("sync", oT[h, :, qb * 512:(qb + 1) * 512], ob_[0:64, :], "st_obf%d" % (qb % 2), [obk], ["oT"])
            P.barrier()

        x1T = dscr("x1T", [1024, NO], F32)
        if "D" in phases:
            AR.reset()
            stage = AR.alloc(1024)
            Wg = AR.alloc(8 * 2048, BF16).rearrange("p (k c) -> p k c", k=8)
            Wglu = AR.alloc(4 * 1024, BF16).rearrange("p (k c) -> p k c", k=4)
            Wos = AR.alloc(4 * 1024, BF16).rearrange("p (k c) -> p k c", k=4)
            Woa = AR.alloc(8 * 1024, BF16).rearrange("p (k c) -> p k c", k=8)
            Wout = AR.alloc(8 * 1024, BF16).rearrange("p (k c) -> p k c", k=8)
            load_scaled_w(Wg, w_in, 8, 2048, gmix, "gmix", "Wg", stage, "stage", c0=928)
            load_scaled_w(Wglu, w_glu, 4, 1024, None, None, "Wglu", stage, "stage")
            load_scaled_w(Wos, w_o_ssm, 4, 1024, None, None, "Wos", stage, "stage")
            for h in range(8):
                dma("sync", stage[0:64, :], w_o_attn[h * 64:(h + 1) * 64, :], "d_stage", (), ["stage"])
                cp(Woa[0:64, h, :], stage[0:64, :], ["stage"], ["Woa"])
            load_scaled_w(Wout, w_out, 8, 1024, None, None, "Wout", stage, "stage")
            xf = AR.alloc(8 * 512).rearrange("p (k t) -> p k t", k=8)
            xb = AR.alloc(8 * 512, BF16).rearrange("p (k t) -> p k t", k=8)
            sq = AR.alloc(8 * 512, BF16).rearrange("p (k t) -> p k t", k=8)
            rstd = AR.alloc(512)
            ysb_ = AR.alloc(4 * 512, BF16).rearrange("p (k t) -> p k t", k=4)
            glu = AR.alloc(4 * 512, BF16).rearrange("p (k t) -> p k t", k=4)
            otb = AR.alloc(8 * 512, BF16).rearrange("p (k t) -> p k t", k=8)
            sg = [AR.alloc(512) for _ in range(2)]
            mixed = AR.alloc(8 * 512, BF16).rearrange("p (k t) -> p k t", k=8)
            x1 = AR.alloc(8 * 512).rearrange("p (k t) -> p k t", k=8)
            import os
            DSTOP = int(os.environ.get("DSTOP", "99"))
            for blk in range(NBO if DSTOP > 0 else 0):
                tg = "D"
                x_block(blk, xf, xb, sq, rstd, tg)
                dma("scalar", ysb_, ysT[:, blk * 512:(blk + 1) * 512].rearrange("(k p) t -> p k t", p=128), "d_ysb", ["ysT"], ["ysb_"])
                dma("scalar", otb[0:64], oT[:, :, blk * 512:(blk + 1) * 512].rearrange("h p t -> p h t"), "d_otb", ["oT"], ["otb"])
                if DSTOP <= 1:
                    continue
                for j in range(4):
                    pv, pkv = psb()
                    pg, pkg = psb()
                    for k in range(4):
                        mm(pv[:, :], Wglu[:, k, j * 128:(j + 1) * 128], ysb_[:, k, :], k == 0, k == 3, ["Wglu", "ysb_"], [pkv])
                    for k in range(4):
                        mm(pg[:, :], Wglu[:, k, 512 + j * 128:512 + (j + 1) * 128], ysb_[:, k, :], k == 0, k == 3, ["Wglu", "ysb_"], [pkg])
                    act(sg[0], pg[:, :], AF.Sigmoid, [pkg], ["sg0"])
                    tt(glu[:, j, :], pv[:, :], sg[0], ALU.mult, [pkv, "sg0"], ["glu"])
                if DSTOP <= 2:
                    continue
                for j in range(8):
                    pa, pka = psb(); ps_, pks = psb(); p0, pk0 = psb(); p1, pk1 = psb()
                    DVAR = int(os.environ.get("DVAR", "0"))
                    hs = [0, 2, 4, 6] if DVAR == 1 else list(range(8))
                    for h in hs:
                        mm(pa[:, :], Woa[0:64, h, j * 128:(j + 1) * 128], otb[0:64, h, :], h == hs[0], h == hs[-1], ["Woa", "otb"], [pka])
                    for k in range(4):
                        mm(ps_[:, :], Wos[:, k, j * 128:(j + 1) * 128], glu[:, k, :], k == 0, k == 3, ["Wos", "glu"], [pks])
                    for k in range(8):
                        mm(p0[:, :], Wg[:, k, j * 128:(j + 1) * 128], xb[:, k, :], k == 0, k == 7, ["Wg", "xb" + tg], [pk0])
                    for k in range(8):
                        mm(p1[:, :], Wg[:, k, 1024 + j * 128:1024 + (j + 1) * 128], xb[:, k, :], k == 0, k == 7, ["Wg", "xb" + tg], [pk1])
                    tt(sg[0], p0[:, :], rstd, ALU.mult, [pk0, "rstd" + tg], ["sg0"])
                    act(sg[0], sg[0], AF.Sigmoid, ["sg0"], ["sg0"])
                    tt(sg[1], p1[:, :], rstd, ALU.mult, [pk1, "rstd" + tg], ["sg1"])
                    act(sg[1], sg[1], AF.Sigmoid, ["sg1"], ["sg1"])
                    tt(sg[0], sg[0], pa[:, :], ALU.mult, ["sg0", pka], ["sg0"])
                    tt(sg[1], sg[1], ps_[:, :], ALU.mult, ["sg1", pks], ["sg1"])
                    tt(mixed[:, j, :], sg[0], sg[1], ALU.add, ["sg0", "sg1"], ["mixed"])
                if DSTOP <= 3:
                    continue
                for j in range(8):
                    pb, pk = psb()
                    for k in range(8):
                        mm(pb[:, :], Wout[:, k, j * 128:(j + 1) * 128], mixed[:, k, :], k == 0, k == 7, ["Wout", "mixed"], [pk])
                    tt(x1[:, j, :], xf[:, j, :], pb[:, :], ALU.add, ["xf" + tg, pk], ["x1"])
                dma("sync", x1T[:, blk * 512:(blk + 1) * 512].rearrange("(k p) t -> p k t", p=128), x1, "st_x1", ["x1"], ["x1T"])
            P.barrier()

        if "E" in phases:
            AR.reset()
            DELTA = 2e-5
            Kbd = AR.alloc(8 * 256, BF16).rearrange("p (h c) -> p h c", h=8)
            x1 = AR.alloc(8 * 512).rearrange("p (k t) -> p k t", k=8)
            rstd = AR.alloc(512)
            h2 = AR.alloc(8 * 512, BF16).rearrange("p (k t) -> p k t", k=8)
            sc = AR.alloc(4 * 2048).rearrange("p (t c) -> p t c", t=4)
            wk = AR.alloc(256)
            tv = AR.alloc(16 * 16).rearrange("p (g k) -> p g k", g=16)
            best = AR.alloc(8 * 16).rearrange("p (h k) -> p h k", h=8)
            eb = AR.alloc(8 * 16).rearrange("p (h k) -> p h k", h=8)
            zs = AR.alloc(8)
            lnz = AR.alloc(4 * 8).rearrange("p (t h) -> p t h", t=4)
            thr = AR.alloc(4 * 8).rearrange("p (t h) -> p t h", t=4)
            wd_off = AR.off
            WdT = [AR.alloc(8 * 512, BF16).rearrange("p (k c) -> p k c", k=8) for _ in range(2)]
            Wu = [AR.alloc(4 * 1024, BF16).rearrange("p (k c) -> p k c", k=4) for _ in range(2)]
            T1f = [AR.alloc(4096) for _ in range(2)]
            T1 = [t.rearrange("p (h a b) -> p h a b", h=8, a=4) for t in T1f]
            T1k = ["T1_0", "T1_1"]
            stage = T1f[0][:, 0:1024]
            cand = T1f[0][:, 0:2048].rearrange("p (h c) -> p h c", h=8)
            Wqy = AR.ap[:, wd_off:wd_off + 4096].bitcast(BF16).rearrange("p (k c) -> p k c", k=8)
            Ef = [AR.alloc(4096, BF16) for _ in range(2)]
            E_ = [t.rearrange("p (h c) -> p h c", h=8) for t in Ef]
            Ek = ["E_0", "E_1"]
            qT = Ef[0].rearrange("p (k t) -> p k t", k=8)
            sq = Ef[1].rearrange("p (k t) -> p k t", k=8)
            Em = AR.alloc(4096, BF16).rearrange("p (h c) -> p h c", h=8)
            gelT = [AR.alloc(512, BF16) for _ in range(3)]
            zT = [AR.alloc(512, BF16) for _ in range(2)]
            zb = [AR.alloc(512, BF16) for _ in range(2)]
            yacc = AR.alloc(4 * 1024).rearrange("p (t c) -> p t c", t=4)
            mset(Kbd, 0.0, ["Kbd"])
            for h in range(8):
                dma("sync", stage[:, 0:128].rearrange("p (n d) -> p n d", n=2), sub_keys[h].rearrange("n k d -> k n d"), "d_stage", (), ["T1_0"])
                pb, pk = psb()
                tr(pb[:, 0:128], stage[:, 0:128], ident_f, ["T1_0", "ident_f"], [pk])
                cp(Kbd[0:64, h, 0:128], pb[0:64, 0:128], [pk], ["Kbd"])
                cp(Kbd[64:128, h, 128:256], pb[64:128, 0:128], [pk], ["Kbd"])
            tg = "E"
            for blk in range(NBO):
                dma("sync", x1, x1T[:, blk * 512:(blk + 1) * 512].rearrange("(k p) t -> p k t", p=128), "d_x1", ["x1T"], ["x1"])
                dma("gpsimd", Wqy, w_query.rearrange("(k p) c -> p k c", p=128), "d_Wqy", (), [("WdT", 0), ("WdT", 1)])
                act(sq, x1, AF.Square, ["x1"], ["E_1"])
                norm_rstd([sq[:, k, :] for k in range(8)], 1024.0, rstd, ["E_1"], "rstd" + tg)
                for k in range(8):
                    stt(h2[:, k, :], x1[:, k, :], gffn[:, k:k + 1], rstd, ALU.mult, ALU.mult, ["x1", "gffn", "rstd" + tg], ["h2"])
                for j in range(8):
                    pb, pk = psb()
                    for k in range(8):
                        mm(pb[:, :], Wqy[:, k, j * 128:(j + 1) * 128], h2[:, k, :], k == 0, k == 7, [("WdT", 0), ("WdT", 1), "h2"], [pk])
                    cp(qT[:, j, :], pb[:, :], [pk], ["E_0"], eng="scalar")
                for t_ in range(4):
                    for hp in range(4):
                        pb, pk = psb()
                        for hh in range(2):
                            h = hp * 2 + hh
                            mm(pb[:, hh * 256:(hh + 1) * 256], qT[:, h, t_ * 128:(t_ + 1) * 128], Kbd[:, h, :], True, True, ["E_0", "Kbd"], [pk])
                        cp(sc[:, t_, hp * 512:(hp + 1) * 512], pb[:, :], [pk], ["sc"], eng="scalar")
                    sc3 = sc[:, t_, :].rearrange("p (g k) -> p g k", g=16)
                    for g in range(16):
                        P.op("vector", (lambda g, sc3: lambda e: e.max(out=tv[:, g, 0:8], in_=sc3[:, g, :]))(g, sc3), ["sc"], ["tv"])
                        P.op("vector", (lambda g, sc3: lambda e: e.match_replace(out=wk[:, 0:128], in_to_replace=tv[:, g, 0:8], in_values=sc3[:, g, :], imm_value=-1e30))(g, sc3), ["sc", "tv"], ["wk"])
                        P.op("vector", (lambda g: lambda e: e.max(out=tv[:, g, 8:16], in_=wk[:, 0:128]))(g), ["wk"], ["tv"])
                    tv4 = tv.rearrange("p (h n) k -> p h n k", n=2)
                    tt(cand.rearrange("p h (a b) -> p h a b", a=16), tv4[:, :, 0, :].unsqueeze(3).to_broadcast([128, 8, 16, 16]),
                       tv4[:, :, 1, :].unsqueeze(2).to_broadcast([128, 8, 16, 16]), ALU.add, ["tv"], ["T1_0"])
                    for h in range(8):
                        P.op("vector", (lambda h: lambda e: e.max(out=best[:, h, 0:8], in_=cand[:, h, :]))(h), ["T1_0"], ["best"])
                        P.op("vector", (lambda h: lambda e: e.match_replace(out=wk[:, 0:256], in_to_replace=best[:, h, 0:8], in_values=cand[:, h, :], imm_value=-1e30))(h), ["T1_0", "best"], ["wk"])
                        P.op("vector", (lambda h: lambda e: e.max(out=best[:, h, 8:16], in_=wk[:, 0:256]))(h), ["wk"], ["best"])
                    tt(eb, best, best[:, :, 15:16].to_broadcast([128, 8, 16]), ALU.subtract, ["best"], ["eb"])
                    act(eb, eb, AF.Exp, ["eb"], ["eb"])
                    P.op("vector", lambda e: e.tensor_reduce(out=zs, in_=eb, axis=AX.X, op=ALU.add), ["eb"], ["zs"])
                    act(lnz[:, t_, :], zs, AF.Ln, ["zs"], ["lnz"])
                    sc4 = sc[:, t_, :].rearrange("p (h n k) -> p h n k", h=8, n=2)
                    tt(sc4[:, :, 0, :], sc4[:, :, 0, :], best[:, :, 15:16].to_broadcast([128, 8, 128]), ALU.subtract, ["sc", "best"], ["sc"])
                    ts(sc4[:, :, 0, :], sc4[:, :, 0, :], DELTA, ALU.add, ["sc"], ["sc"])
                    ts(thr[:, t_, :], lnz[:, t_, :], -1.0, ALU.mult, ["lnz"], ["thr"], s2=-DELTA, op1=ALU.add)
                    mset(yacc[:, t_, :], 0.0, ["yacc"])
                its = [(ec, t_) for ec in range(32) for t_ in range(4)]

                NIT = len(its)
                PA = [(pbs[0], pbk[0]), (pbs[1], pbk[1])]
                PG = [(pbs[2], pbk[2]), (pbs[3], pbk[3])]
                PZ = [(pbs[4], pbk[4]), (pbs[5], pbk[5])]
                PY = [(pbs[6], pbk[6]), (pbs[7], pbk[7])]

                def opA(n):
                    ec, t_ = its[n]
                    s = ec % 2
                    if t_ == 0:
                        dma("gpsimd", WdT[s], w_downT[:, ec * 512:(ec + 1) * 512].rearrange("(k p) c -> p k c", p=128), "d_WdT%d" % s, (), [("WdT", s)])
                        dma("gpsimd", Wu[s], w_up[ec * 512:(ec + 1) * 512, :].rearrange("(k p) c -> p k c", p=128), "d_Wu%d" % s, (), [("Wu", s)])
                    pa, pka = PA[n % 2]
                    for k in range(8):
                        mm(pa[:, :], h2[:, k, t_ * 128:(t_ + 1) * 128], WdT[s][:, k, :], k == 0, k == 7, ["h2", ("WdT", s)], [pka])

                def opGelu(n):
                    pa, pka = PA[n % 2]
                    act(gelT[n % 3], pa[:, :], AF.Gelu_apprx_tanh, [pka], [("gelT", n % 3)])

                def opT1(n):
                    ec, t_ = its[n]
                    i = n % 2
                    sc4 = sc[:, t_, :].rearrange("p (h n k) -> p h n k", h=8, n=2)
                    tt(T1[i], sc4[:, :, 0, ec * 4:(ec + 1) * 4].unsqueeze(3).to_broadcast([128, 8, 4, 128]),
                       sc4[:, :, 1, :].unsqueeze(2).to_broadcast([128, 8, 4, 128]), ALU.add, ["sc"], [T1k[i]])

                def opExp(n):
                    ec, t_ = its[n]
                    i = n % 2
                    T1h = T1f[i].rearrange("p (h c) -> p h c", h=8)
                    for h in range(8):
                        act(E_[i][:, h, :], T1h[:, h, :], AF.Exp, [T1k[i], "thr"], [Ek[i]], bias=thr[:, t_, h:h + 1])

                def opSTT(n):
                    i = n % 2
                    stt(Em.rearrange("p h c -> p (h c)"), T1f[i], 0.0, Ef[i], ALU.is_ge, ALU.mult, [T1k[i], Ek[i]], [("Em", h) for h in range(8)])

                def opG(n):
                    pg, pkg = PG[n % 2]
                    for h in range(8):
                        mm(pg[:, :], ident_b, Em[:, h, :], h == 0, h == 7, [("Em", h), "ident_b"], [pkg])

                def opZb(n):
                    pg, pkg = PG[n % 2]
                    tt(zb[n % 2], gelT[n % 3], pg[:, :], ALU.mult, [("gelT", n % 3), pkg], [("zb", n % 2)])

                def opTr(n):
                    pz, pkz = PZ[n % 2]
                    pzb = pz[:, 0:256].bitcast(BF16).rearrange("p (q t) -> p q t", q=4)
                    for q in range(4):
                        tr(pzb[:, q, :], zb[n % 2][:, q * 128:(q + 1) * 128], ident_b, [("zb", n % 2), "ident_b"], [pkz])

                def opEvac(n):
                    pz, pkz = PZ[n % 2]
                    pzb = pz[:, 0:256].bitcast(BF16).rearrange("p (q t) -> p q t", q=4)
                    cp(zT[n % 2].rearrange("p (q t) -> p q t", q=4), pzb, [pkz], [("zT", n % 2)], eng="scalar")

                def opY(n):
                    ec, t_ = its[n]
                    s = ec % 2
                    z3 = zT[n % 2].rearrange("p (q t) -> p q t", q=4)
                    for hf in range(2):
                        py, pky = PY[hf]
                        for q in range(4):
                            mm(py[:, :], z3[:, q, :], Wu[s][:, q, hf * 512:(hf + 1) * 512], q == 0, q == 3, [("zT", n % 2), ("Wu", s)], [pky])

                def opYacc(n):
                    ec, t_ = its[n]
                    for hf in range(2):
                        py, pky = PY[hf]
                        tt(yacc[:, t_, hf * 512:(hf + 1) * 512], yacc[:, t_, hf * 512:(hf + 1) * 512], py[:, :], ALU.add, ["yacc", pky], ["yacc"])

                def ok(n):
                    return 0 <= n < NIT

                for i in range(NIT + 3):
                    if ok(i):
                        opA(i); opGelu(i); opT1(i); opExp(i)
                    if ok(i - 1):
                        opSTT(i - 1)
                    if ok(i - 3):
                        opY(i - 3); opYacc(i - 3)
                    if ok(i - 1):
                        opG(i - 1)
                    if ok(i - 2):
                        opZb(i - 2); opTr(i - 2); opEvac(i - 2)
                for t_ in range(4):
                    for j in range(8):
                        pb, pk = psb()
                        tr(pb[:, 0:128], yacc[:, t_, j * 128:(j + 1) * 128], ident_f, ["yacc", "ident_f"], [pk])
                        tt(x1[:, j, t_ * 128:(t_ + 1) * 128], x1[:, j, t_ * 128:(t_ + 1) * 128], pb[:, 0:128], ALU.add, ["x1", pk], ["x1"])
                act(sq, x1, AF.Square, ["x1"], ["E_1"])
                norm_rstd([sq[:, k, :] for k in range(8)], 1024.0, rstd, ["E_1"], "rstd" + tg)
                for k in range(8):
                    stt(x1[:, k, :], x1[:, k, :], gfin[:, k:k + 1], rstd, ALU.mult, ALU.mult, ["x1", "gfin", "rstd" + tg], ["x1"])
                dma("sync", outT[:, blk * 512:(blk + 1) * 512].rearrange("(k p) t -> p k t", p=128), x1, "st_out", ["x1"], ["outT"])
        P.emit(final_wait_keys=[k for k in ("st_out",) if "E" in phases])
    return nc, len(P.ops)


_CACHE = {}


def _consts():
    ident = np.eye(128, dtype=np.float32)
    i64 = np.eye(64, dtype=np.float32)
    i2 = np.block([[i64, i64], [i64, i64]]).astype(np.float32)
    return ident, i2


def make_in_maps(inp, S):
    B = inp["x"].shape[0]
    NO = S // 2
    ident, i2 = _consts()
    w_downT = np.ascontiguousarray(inp["w_down"][0].T)
    maps = []
    for c in range(2 * B):
        b, half = c // 2, c % 2
        xb = inp["x"][b]
        if half:
            xb = xb[::-1]
        pinfo = np.empty((128, 2), dtype=np.float32)
        pinfo[:, 0] = -1.0 if half else 1.0
        pinfo[:, 1] = float(S - 1) if half else 0.0
        dsel = [1, 0] if half else [0, 1]
        m = dict(
            xT=np.ascontiguousarray(xb.T), pinfo=pinfo,
            norm_mix=inp["norm_mix"][0], w_in=inp["w_in"][0], q_a_norm=inp["q_a_norm"][0], w_q_b=inp["w_q_b"][0],
            kv_a_norm=inp["kv_a_norm"][0], w_kv_b=inp["w_kv_b"][0], w_o_attn=inp["w_o_attn"][0],
            lam_re=np.ascontiguousarray(inp["lam_re"][0][dsel]), lam_im=np.ascontiguousarray(inp["lam_im"][0][dsel]),
            log_dt=np.ascontiguousarray(inp["log_dt"][0][dsel].reshape(64)),
            b_re=np.ascontiguousarray(inp["b_re"][0][dsel]), b_im=np.ascontiguousarray(inp["b_im"][0][dsel]),
            c_re=np.ascontiguousarray(inp["c_re"][0][dsel]), c_im=np.ascontiguousarray(inp["c_im"][0][dsel]),
            d_skip=inp["d_skip"][0], w_glu=inp["w_glu"][0], w_o_ssm=inp["w_o_ssm"][0], w_out=inp["w_out"][0],
            norm_ffn=inp["norm_ffn"][0], w_query=inp["w_query"][0], sub_keys=inp["sub_keys"][0],
            w_downT=w_downT, w_up=inp["w_up"][0], final_norm=inp["final_norm"], ident=ident, i2c=i2,
        )
        maps.append({k: np.ascontiguousarray(np.asarray(v, dtype=np.float32)) for k, v in m.items()})
    return maps


def kernel(**inputs):
    inp = {k: np.asarray(v) for k, v in inputs.items()}
    B, S, D = inp["x"].shape
    NO = S // 2
    if S not in _CACHE:
        _CACHE[S] = build(S)[0]
    nc = _CACHE[S]
    maps = make_in_maps(inp, S)
    res = run_bass_kernel_spmd(nc, maps, core_ids=list(range(2 * B)))
    out = np.empty((B, S, D), dtype=np.float32)
    for c in range(2 * B):
        b, half = c // 2, c % 2
        o = np.asarray(res.results[c]["outT"]).T
        if half == 0:
            out[b, :NO] = o
        else:
            out[b, NO:] = o[::-1]
    return out
```

```python
from contextlib import ExitStack
import numpy as np
import ml_dtypes
import concourse.bass as bass
import concourse.mybir as mybir
from concourse.bass_utils import run_bass_kernel_spmd

F32 = mybir.dt.float32
BF16 = mybir.dt.bfloat16
AF = mybir.ActivationFunctionType
ALU = mybir.AluOpType
AX = mybir.AxisListType
SEM_CAP = 30000
EPS = 1e-6


class Prog:
    ENGS = ("sync", "scalar", "vector", "gpsimd", "tensor")

    def __init__(self, nc):
        self.nc = nc
        self.ops = []

    def op(self, eng, fn, reads=(), writes=(), dma_key=None):
        xs = [k for k in reads if isinstance(k, tuple) and k and k[0] == "pb"]
        if xs:
            writes = tuple(writes) + tuple(k for k in xs if k not in writes)
        self.ops.append(dict(eng=eng, fn=fn, reads=tuple(reads), writes=tuple(writes),
                             dma_key=dma_key, waits=[], inc=None, idx=len(self.ops), bar=False))

    def dma(self, eng, fn, key, reads=(), writes=()):
        self.op(eng, fn, reads, writes, dma_key=key)

    def barrier(self):
        self.ops.append(dict(bar=True, idx=len(self.ops)))

    def analyze(self):
        last_w, readers = {}, {}
        eng_cnt = {e: 0 for e in self.ENGS}
        last_sig = {}
        dma_cnt = {}
        waited = {e: {} for e in self.ENGS}
        pending = {e: [] for e in self.ENGS}
        for o in self.ops:
            if o["bar"]:
                pend = list(last_sig.values()) + [(("dma", k), v) for k, v in dma_cnt.items()]
                for e in self.ENGS:
                    pending[e] = list(pend)
                continue
            e = o["eng"]
            deps = set()
            for k in o["reads"]:
                if k in last_w:
                    deps.add(last_w[k])
            for k in o["writes"]:
                if k in last_w:
                    deps.add(last_w[k])
                for r in readers.get(k, ()):
                    deps.add(r)
            deps.discard(o["idx"])
            wl = list(pending[e])
            pending[e] = []
            for d in sorted(deps):
                p = self.ops[d]
                if p["dma_key"] is None and p["eng"] == e and e == "tensor":
                    continue
                if p["dma_key"] is not None:
                    wl.append((("dma", p["dma_key"]), dma_cnt[p["dma_key"]]))
                else:
                    wl.append(p["sig"])
            for sem, val in wl:
                if waited[e].get(sem, 0) >= val:
                    continue
                waited[e][sem] = val
                o["waits"].append((sem, val))
            if o["dma_key"] is not None:
                dma_cnt[o["dma_key"]] = dma_cnt.get(o["dma_key"], 0) + 16
                o["inc"] = (("dma", o["dma_key"]), 16)
            else:
                c = eng_cnt[e]
                sem = ("eng", e, c // SEM_CAP)
                eng_cnt[e] = c + 1
                o["sig"] = (sem, c % SEM_CAP + 1)
                o["inc"] = (sem, 1)
                last_sig[e] = o["sig"]
            for k in o["reads"]:
                readers.setdefault(k, []).append(o["idx"])
            for k in o["writes"]:
                last_w[k] = o["idx"]
                readers[k] = []
        self.dma_cnt = dma_cnt
        sems = []
        seen = set()
        for o in self.ops:
            if o["bar"]:
                continue
            if o["inc"][0] not in seen:
                seen.add(o["inc"][0])
                sems.append(o["inc"][0])
        self.sem_names = sems

    def emit(self, final_wait_keys=()):
        nc = self.nc
        self.analyze()
        with ExitStack() as es:
            semobj = {}
            for i, s in enumerate(self.sem_names):
                semobj[s] = es.enter_context(nc.semaphore("s%d" % i))
            block = es.enter_context(nc.Block())
            ops = [o for o in self.ops if not o["bar"]]
            dma_cnt = self.dma_cnt

            def gen(ename):
                def body(eng):
                    for o in ops:
                        if o["eng"] != ename:
                            continue
                        for (s, v) in o["waits"]:
                            eng.wait_ge(semobj[s], v)
                        ins = o["fn"](eng)
                        s, n = o["inc"]
                        ins.then_inc(semobj[s], n)
                    if ename == "sync":
                        for k in final_wait_keys:
                            eng.wait_ge(semobj[("dma", k)], dma_cnt[k])
                return body

            block.sync(gen("sync"))
            block.scalar(gen("scalar"))
            block.vector(gen("vector"))
            block.gpsimd(gen("gpsimd"))
            block.tensor(gen("tensor"))


class Arena:
    def __init__(self, ap, ncols):
        self.ap, self.n, self.off = ap, ncols, 0

    def reset(self):
        self.off = 0

    def alloc(self, cols, dt=F32):
        w = cols if dt == F32 else (cols + 1) // 2
        a = self.ap[:, self.off:self.off + w]
        self.off += w
        assert self.off <= self.n, ("arena overflow", self.off)
        return a if dt == F32 else a.bitcast(dt)


def build(S, phases="ABCDE"):
    NO = S // 2
    NB = S // 512
    NBO = NO // 512
    NKT = S // 128
    nc = bass.Bass("TRN2", target_bir_lowering=False)

    def din(name, shape, dt=F32):
        return nc.dram_tensor(name, list(shape), dt, kind="ExternalInput").ap()

    def dscr(name, shape, dt):
        return nc.dram_tensor(name, list(shape), dt, kind="Internal").ap()

    xT = din("xT", [1024, S])
    pinfo = din("pinfo", [128, 2])
    cosT = nc.dram_tensor("cosS", [32, S], F32, kind="Internal").ap()
    sinT = nc.dram_tensor("sinS", [32, S], F32, kind="Internal").ap()
    norm_mix = din("norm_mix", [1024])
    w_in = din("w_in", [1024, 2976])
    q_a_norm = din("q_a_norm", [256])
    w_q_b = din("w_q_b", [256, 768])
    kv_a_norm = din("kv_a_norm", [128])
    w_kv_b = din("w_kv_b", [128, 1024])
    w_o_attn = din("w_o_attn", [512, 1024])
    lam_re = din("lam_re", [2, 32, 64])
    lam_im = din("lam_im", [2, 32, 64])
    log_dt = din("log_dt", [64])
    b_re = din("b_re", [2, 32, 64, 16])
    b_im = din("b_im", [2, 32, 64, 16])
    c_re = din("c_re", [2, 32, 16, 64])
    c_im = din("c_im", [2, 32, 16, 64])
    d_skip = din("d_skip", [512])
    w_glu = din("w_glu", [512, 1024])
    w_o_ssm = din("w_o_ssm", [512, 1024])
    w_out = din("w_out", [1024, 1024])
    norm_ffn = din("norm_ffn", [1024])
    w_query = din("w_query", [1024, 1024])
    sub_keys = din("sub_keys", [8, 2, 128, 64])
    w_downT = din("w_downT", [1024, 16384])
    w_up = din("w_up", [16384, 1024])
    final_norm = din("final_norm", [1024])
    ident_in = din("ident", [128, 128])
    i2_in = din("i2c", [128, 128])
    outT = nc.dram_tensor("outT", [1024, NO], F32, kind="ExternalOutput").ap()

    latT = dscr("latT", [128, S], BF16)
    krT = dscr("krT", [32, S], BF16)
    uT = dscr("uT", [512, S], BF16)
    hqT = dscr("hqT", [256, NO], BF16)
    ysT = dscr("ysT", [512, NO], BF16)
    oT = dscr("oT", [8, 64, NO], BF16)

    P = Prog(nc)
    es = ExitStack()
    with es:
        arena_t = es.enter_context(nc.sbuf_tensor("arena", [128, 45600], F32))
        consts = es.enter_context(nc.sbuf_tensor("consts", [128, 1024], F32))
        AR = Arena(arena_t, 45600)
        pbs = [es.enter_context(nc.psum_tensor("pb%d" % i, [128, 512], F32)) for i in range(6)]
        pby2 = es.enter_context(nc.psum_tensor("pby2", [128, 1024], F32))
        pbs = pbs + [pby2[:, 0:512], pby2[:, 512:1024]]
        pbk = [("pb", i) for i in range(8)]

        def mm(out, lhsT, rhs, start, stop, reads, writes):
            P.op("tensor", lambda e: e.matmul(out, lhsT=lhsT, rhs=rhs, start=start, stop=stop), reads, writes)

        def tr(out, in_, ident, reads, writes):
            P.op("tensor", lambda e: e.transpose(out, in_, ident), reads, writes)

        def act(out, in_, func, reads, writes, bias=None, scale=None):
            kw = {}
            if bias is not None:
                kw["bias"] = bias
            if scale is not None:
                kw["scale"] = scale
            P.op("scalar", lambda e: e.activation(out=out, in_=in_, func=func, **kw), reads, writes)

        def tt(out, in0, in1, op, reads, writes, eng="vector"):
            P.op(eng, lambda e: e.tensor_tensor(out=out, in0=in0, in1=in1, op=op), reads, writes)

        def ts(out, in0, s1, op0, reads, writes, s2=None, op1=None, eng="vector"):
            if op1 is None:
                P.op(eng, lambda e: e.tensor_scalar(out=out, in0=in0, scalar1=s1, scalar2=None, op0=op0), reads, writes)
            else:
                P.op(eng, lambda e: e.tensor_scalar(out=out, in0=in0, scalar1=s1, scalar2=s2, op0=op0, op1=op1), reads, writes)

        def stt(out, in0, scalar, in1, op0, op1, reads, writes):
            P.op("vector", lambda e: e.scalar_tensor_tensor(out=out, in0=in0, scalar=scalar, in1=in1, op0=op0, op1=op1), reads, writes)

        def cp(out, in_, reads, writes, eng="vector"):
            if eng == "scalar":
                act(out, in_, AF.Copy, reads, writes)
            else:
                P.op(eng, lambda e: e.tensor_copy(out=out, in_=in_), reads, writes)

        def mset(ap, val, writes, eng="vector"):
            P.op(eng, lambda e: e.memset(ap, val), (), writes)

        def recip(out, in_, reads, writes):
            P.op("vector", lambda e: e.reciprocal(out=out, in_=in_), reads, writes)

        def dma(eng, out, in_, key, reads, writes, slow=False):
            if slow:
                P.dma(eng, lambda e: e.dma_start(out=out, in_=in_, allow_slow_non_contiguous=True), key, reads, writes)
            else:
                P.dma(eng, lambda e: e.dma_start(out=out, in_=in_), key, reads, writes)

        ident_f = consts[:, 0:128]
        i2c = consts[:, 128:256]
        ident_b = consts[:, 256:320].bitcast(BF16)
        ones_b = consts[:, 320:384].bitcast(BF16)
        ones_f = consts[:, 384:512]
        gmix = consts[:, 512:520]
        gffn = consts[:, 520:528]
        gfin = consts[:, 528:536]
        gq = consts[:, 536:538]
        gkv = consts[:, 538:539]
        dsk = consts[:, 540:544]
        epsc = consts[:, 544:545]
        dma("sync", ident_f, ident_in, "c_id", (), ["ident_f"])
        dma("sync", i2c, i2_in, "c_i2", (), ["i2c"])
        cp(ident_b, ident_f, ["ident_f"], ["ident_b"])
        mset(ones_b, 1.0, ["ones_b"])
        mset(ones_f, 1.0, ["ones_f"])
        mset(epsc, EPS, ["epsc"])
        for nm, dst, src, k in (("gmix", gmix, norm_mix, 8), ("gffn", gffn, norm_ffn, 8), ("gfin", gfin, final_norm, 8),
                                ("gq", gq, q_a_norm, 2), ("gkv", gkv, kv_a_norm, 1), ("dsk", dsk, d_skip, 4)):
            dma("sync", dst, src.rearrange("(k p) -> p k", p=128), "c_" + nm, (), [nm], slow=True)

        rr = {"ps": 0}

        def psb(n=1):
            i = rr["ps"] % 8
            rr["ps"] += 1
            return pbs[i], pbk[i]

        def norm_rstd(sq_ap_list, nfeat, out_bc, reads, okey, pre=None):
            pb, pk = psb()
            n = len(sq_ap_list)
            for i, a in enumerate(sq_ap_list):
                mm(pb[:, :], ones_b[0:a.shape[0], :], a, i == 0, i == n - 1, reads + ["ones_b"], [pk])
            if pre is None:
                act(out_bc, pb[:, :], AF.Sqrt, [pk, "epsc"], [okey], bias=epsc[:, 0:1], scale=1.0 / nfeat)
            else:
                tt(out_bc, pb[:, :], pre[0], ALU.mult, [pk, pre[1]], [okey])
                tt(out_bc, out_bc, pre[0], ALU.mult, [okey, pre[1]], [okey])
                act(out_bc, out_bc, AF.Sqrt, [okey, "epsc"], [okey], bias=epsc[:, 0:1], scale=1.0 / nfeat)
            recip(out_bc, out_bc, [okey], [okey])

        def load_scaled_w(dst_b, src, kt_n, ncols, gtile, gkey, key, stage, skey, c0=0):
            for kt in range(kt_n):
                for c in range(0, ncols, 1024):
                    w = min(1024, ncols - c)
                    dma("sync", stage[:, 0:w], src[kt * 128:(kt + 1) * 128, c0 + c:c0 + c + w], "d_" + skey, (), [skey])
                    if gtile is None:
                        cp(dst_b[:, kt, c:c + w], stage[:, 0:w], [skey], [key])
                    else:
                        ts(dst_b[:, kt, c:c + w], stage[:, 0:w], gtile[:, kt:kt + 1], ALU.mult, [skey, gkey], [key])

        def x_block(blk, xf, xb, sq, rstd, tag):
            dma("sync", xf, xT[:, blk * 512:(blk + 1) * 512].rearrange("(k p) t -> p k t", p=128), "d_xf" + tag, (), ["xf" + tag])
            cp(xb, xf, ["xf" + tag], ["xb" + tag], eng="gpsimd")
            act(sq, xf, AF.Square, ["xf" + tag], ["sq" + tag])
            norm_rstd([sq[:, k, :] for k in range(8)], 1024.0, rstd, ["sq" + tag], "rstd" + tag)

        if "A" in phases or "C" in phases:
            I32 = mybir.dt.int32
            CW = S // 4
            p0_base = AR.n - (5 * CW + 16)
            AR.off = p0_base
            pin = AR.alloc(2)
            dma("sync", pin, pinfo, "d_pin", (), ["pin"])
            ji = AR.alloc(1).bitcast(I32)
            jf = AR.alloc(1); jm = AR.alloc(1); ifr = AR.alloc(1)
            for g in range(4):
                P.op("gpsimd", (lambda g: lambda e: e.iota(ji[g * 32:(g + 1) * 32, :], pattern=[[1, 1]], base=0, channel_multiplier=1))(g), (), ["ji"])
            cp(jf, ji, ["ji"], ["jf"])
            ts(jm, jf, 16.0, ALU.is_ge, ["jf"], ["jm"], s2=-16.0, op1=ALU.mult)
            tt(jf, jf, jm, ALU.add, ["jf", "jm"], ["jf"])
            act(ifr, jf, AF.Exp, ["jf"], ["ifr"], scale=float(-np.log(10000.0) / 16.0))
            tbuf = AR.alloc(CW); ang = AR.alloc(CW); kq = AR.alloc(CW); rr_ = AR.alloc(CW); wm_ = AR.alloc(CW)
            ti = tbuf.bitcast(I32); kqi = tbuf.bitcast(I32)
            so = ang; r2 = kq; co = rr_

            def wrap0(o, okey):
                ts(wm_, o, float(-np.pi), ALU.is_lt, [okey], ["wm_"], s2=float(2 * np.pi), op1=ALU.mult)
                tt(o, o, wm_, ALU.add, [okey, "wm_"], [okey])
                ts(wm_, o, float(np.pi), ALU.is_gt, [okey], ["wm_"], s2=float(-2 * np.pi), op1=ALU.mult)
                tt(o, o, wm_, ALU.add, [okey, "wm_"], [okey])

            for g in range(4):
                P.op("gpsimd", (lambda g: lambda e: e.iota(ti[g * 32:(g + 1) * 32, :], pattern=[[1, CW]], base=g * CW, channel_multiplier=0))(g), (), ["tbuf"])
            cp(ang, ti, ["tbuf"], ["ang"])
            ts(ang, ang, pin[:, 0:1], ALU.mult, ["ang", "pin"], ["ang"], s2=pin[:, 1:2], op1=ALU.add)
            ts(ang, ang, ifr[:, 0:1], ALU.mult, ["ang", "ifr"], ["ang"])
            ts(kq, ang, float(1.0 / (2 * np.pi)), ALU.mult, ["ang"], ["kq"], s2=0.5, op1=ALU.add)
            cp(kqi, kq, ["kq"], ["tbuf"])
            cp(kq, kqi, ["tbuf"], ["kq"])
            stt(rr_, kq, -6.28125, ang, ALU.mult, ALU.add, ["kq", "ang"], ["rr_"])
            stt(rr_, kq, float(-(2 * np.pi - 6.28125)), rr_, ALU.mult, ALU.add, ["kq", "rr_"], ["rr_"])
            wrap0(rr_, "rr_")
            act(so, rr_, AF.Sin, ["rr_"], ["ang"])
            ts(r2, rr_, float(np.pi / 2), ALU.add, ["rr_"], ["kq"])
            wrap0(r2, "kq")
            act(co, r2, AF.Sin, ["kq"], ["rr_"])
            for g in range(4):
                dma("sync", sinT[:, g * CW:(g + 1) * CW], so[g * 32:(g + 1) * 32, :], "st_so", ["ang"], ["ropeS"])
                dma("scalar", cosT[:, g * CW:(g + 1) * CW], co[g * 32:(g + 1) * 32, :], "st_co", ["rr_"], ["ropeC"])
            P0_BASE = p0_base

        if "A" in phases:
            AR.reset()
            stage = AR.alloc(1024)
            Wa = AR.alloc(8 * 928, BF16).rearrange("p (k c) -> p k c", k=8)
            Wkr = AR.alloc(8 * 96, BF16).rearrange("p (k c) -> p k c", k=8)
            Wkrr = AR.alloc(8 * 96, BF16).rearrange("p (k c) -> p k c", k=8)
            load_scaled_w(Wa, w_in, 8, 928, gmix, "gmix", "Wa", stage, "stage")
            mset(Wkr, 0.0, ["Wkr"])
            mset(Wkrr, 0.0, ["Wkrr"])
            cp(Wkr[:, :, 64:96], Wa[:, :, 384:416], ["Wa"], ["Wkr"])
            ts(Wkrr[:, :, 64:80], Wa[:, :, 400:416], -1.0, ALU.mult, ["Wa"], ["Wkrr"])
            cp(Wkrr[:, :, 80:96], Wa[:, :, 384:400], ["Wa"], ["Wkrr"])
            xfs = [AR.alloc(8 * 512).rearrange("p (k t) -> p k t", k=8) for _ in range(2)]
            xbs = [AR.alloc(8 * 512, BF16).rearrange("p (k t) -> p k t", k=8) for _ in range(2)]
            sqs = [AR.alloc(8 * 512, BF16).rearrange("p (k t) -> p k t", k=8) for _ in range(2)]
            rstds = [AR.alloc(512) for _ in range(2)]
            cst = [AR.alloc(512) for _ in range(2)]
            snt = [AR.alloc(512) for _ in range(2)]
            tmpf = [AR.alloc(1024) for _ in range(2)]
            tmpb = [AR.alloc(1024, BF16) for _ in range(2)]
            r2 = [AR.alloc(512) for _ in range(2)]
            outb = [AR.alloc(512 * 8, BF16) for _ in range(2)]
            assert AR.off <= P0_BASE, (AR.off, P0_BASE)
            for blk in range(NB):
                s = blk % 2
                tg = "A%d" % s
                xf, xb, sq, rstd = xfs[s], xbs[s], sqs[s], rstds[s]
                x_block(blk, xf, xb, sq, rstd, tg)
                ob = outb[s]
                okey = "outb" + tg
                dma("scalar", cst[s][64:96, :], cosT[:, blk * 512:(blk + 1) * 512], "d_cs" + tg, ["ropeC"], ["cs" + tg])
                dma("scalar", snt[s][64:96, :], sinT[:, blk * 512:(blk + 1) * 512], "d_sn" + tg, ["ropeS"], ["sn" + tg])
                pb, pk = psb()
                for k in range(8):
                    mm(pb[:, :], Wa[:, k, 256:384], xb[:, k, :], k == 0, k == 7, ["Wa", "xb" + tg], [pk])
                tf = tmpf[s]
                tt(tf[:, 0:512], pb[:, :], rstd, ALU.mult, [pk, "rstd" + tg], ["tf" + tg])
                act(tmpb[s][:, 0:512], tf[:, 0:512], AF.Square, ["tf" + tg], ["tb" + tg])
                norm_rstd([tmpb[s][:, 0:512]], 128.0, r2[s], ["tb" + tg], "r2" + tg)
                stt(ob[:, 0:512], tf[:, 0:512], gkv[:, 0:1], r2[s], ALU.mult, ALU.mult, ["tf" + tg, "gkv", "r2" + tg], [okey])
                dma("sync", latT[:, blk * 512:(blk + 1) * 512], ob[:, 0:512], "st_" + okey, [okey], ["latT"])
                pa, pka = psb()
                pb2, pkb = psb()
                for k in range(8):
                    mm(pa[0:96, :], Wkr[:, k, :], xb[:, k, :], k == 0, k == 7, ["Wkr", "xb" + tg], [pka])
                for k in range(8):
                    mm(pb2[0:96, :], Wkrr[:, k, :], xb[:, k, :], k == 0, k == 7, ["Wkrr", "xb" + tg], [pkb])
                tt(tf[64:96, 0:512], pa[64:96, :], cst[s][64:96, :], ALU.mult, [pka, "cs" + tg], ["tf" + tg])
                tt(tf[64:96, 512:1024], pb2[64:96, :], snt[s][64:96, :], ALU.mult, [pkb, "sn" + tg], ["tf" + tg])
                tt(tf[64:96, 0:512], tf[64:96, 0:512], tf[64:96, 512:1024], ALU.add, ["tf" + tg], ["tf" + tg])
                tt(ob[64:96, 512:1024], tf[64:96, 0:512], rstd[64:96, :], ALU.mult, ["tf" + tg, "rstd" + tg], [okey])
                dma("sync", krT[:, blk * 512:(blk + 1) * 512], ob[64:96, 512:1024], "st_" + okey, [okey], ["krT"])
                for j in range(4):
                    pb, pk = psb()
                    for k in range(8):
                        mm(pb[:, :], Wa[:, k, 416 + j * 128:416 + (j + 1) * 128], xb[:, k, :], k == 0, k == 7, ["Wa", "xb" + tg], [pk])
                    tt(ob[:, 1024 + j * 512:1024 + (j + 1) * 512], pb[:, :], rstd, ALU.mult, [pk, "rstd" + tg], [okey])
                dma("sync", uT[:, blk * 512:(blk + 1) * 512].rearrange("(j p) t -> p j t", p=128),
                    ob[:, 1024:3072].rearrange("p (j t) -> p j t", j=4), "st_" + okey, [okey], ["uT"])
                if blk < NBO:
                    for j in range(2):
                        pb, pk = psb()
                        for k in range(8):
                            mm(pb[:, :], Wa[:, k, j * 128:(j + 1) * 128], xb[:, k, :], k == 0, k == 7, ["Wa", "xb" + tg], [pk])
                        tt(tf[:, j * 512:(j + 1) * 512], pb[:, :], rstd, ALU.mult, [pk, "rstd" + tg], ["tf" + tg])
                    act(tmpb[s][:, 0:1024], tf[:, 0:1024], AF.Square, ["tf" + tg], ["tb" + tg])
                    norm_rstd([tmpb[s][:, 0:512], tmpb[s][:, 512:1024]], 256.0, r2[s], ["tb" + tg], "r2" + tg)
                    for j in range(2):
                        stt(ob[:, 3072 + j * 512:3072 + (j + 1) * 512], tf[:, j * 512:(j + 1) * 512], gq[:, j:j + 1], r2[s],
                            ALU.mult, ALU.mult, ["tf" + tg, "gq", "r2" + tg], [okey])
                    dma("sync", hqT[:, blk * 512:(blk + 1) * 512].rearrange("(j p) t -> p j t", p=128),
                        ob[:, 3072:4096].rearrange("p (j t) -> p j t", j=2), "st_" + okey, [okey], ["hqT"])
            P.barrier()

        if "B" in phases:
            AR.reset()
            nsteps_b = S.bit_length() - 1
            nsteps_f = NO.bit_length() - 1
            lre = AR.alloc(64); lim = AR.alloc(64); dtb = AR.alloc(64)
            for hlf in range(2):
                dma("sync", lre[hlf * 64:(hlf + 1) * 64, :], lam_re.rearrange("d g p -> p (d g)"), "d_lre", (), ["lre"], slow=True)
                dma("sync", lim[hlf * 64:(hlf + 1) * 64, :], lam_im.rearrange("d g p -> p (d g)"), "d_lim", (), ["lim"], slow=True)
            dma("sync", dtb, log_dt.partition_broadcast(128), "d_dt", (), ["dtb"])
            act(dtb, dtb, AF.Exp, ["dtb"], ["dtb"])
            al = AR.alloc(64); th = AR.alloc(64); kk = AR.alloc(64); ki = AR.alloc(64).bitcast(mybir.dt.int32)
            tt(al, lre, dtb, ALU.mult, ["lre", "dtb"], ["al"])
            tt(th, lim, dtb, ALU.mult, ["lim", "dtb"], ["th"])
            rmag = AR.alloc(64)
            act(rmag, al, AF.Exp, ["al"], ["rmag"])
            ts(kk, th, 1.0 / (2 * np.pi), ALU.mult, ["th"], ["kk"], s2=0.5, op1=ALU.add)
            cp(ki, kk, ["kk"], ["ki"])
            cp(kk, ki, ["ki"], ["kk"])
            thr = AR.alloc(64)
            stt(thr, kk, -2 * np.pi, th, ALU.mult, ALU.add, ["kk", "th"], ["thr"])
            sn = AR.alloc(64); cs = AR.alloc(64); wr = AR.alloc(64)
            wm = AR.alloc(64)

            def wrap(o, i, shift, okey):
                ts(o, i, float(shift), ALU.add, ["thr"], [okey])
                ts(wm, o, float(-np.pi), ALU.is_lt, [okey], ["wm"], s2=float(2 * np.pi), op1=ALU.mult)
                tt(o, o, wm, ALU.add, [okey, "wm"], [okey])
                ts(wm, o, float(np.pi), ALU.is_gt, [okey], ["wm"], s2=float(-2 * np.pi), op1=ALU.mult)
                tt(o, o, wm, ALU.add, [okey, "wm"], [okey])
            wrap(wr, thr, 0.0, "wr")
            act(sn, wr, AF.Sin, ["wr"], ["sn"])
            wr2 = AR.alloc(64)
            wrap(wr2, thr, np.pi / 2, "wr2")
            act(cs, wr2, AF.Sin, ["wr2"], ["cs"])
            are = AR.alloc(64); aim = AR.alloc(64)
            tt(are, rmag, cs, ALU.mult, ["rmag", "cs"], ["are"])
            tt(aim, rmag, sn, ALU.mult, ["rmag", "sn"], ["aim"])
            den = AR.alloc(64); t1 = AR.alloc(64); t2 = AR.alloc(64); am1 = AR.alloc(64); kre = AR.alloc(64); kim = AR.alloc(64)
            tt(den, lre, lre, ALU.mult, ["lre"], ["den"])
            tt(t1, lim, lim, ALU.mult, ["lim"], ["t1"])
            tt(den, den, t1, ALU.add, ["den", "t1"], ["den"])
            recip(den, den, ["den"], ["den"])
            ts(am1, are, -1.0, ALU.add, ["are"], ["am1"])
            tt(t1, am1, lre, ALU.mult, ["am1", "lre"], ["t1"])
            tt(t2, aim, lim, ALU.mult, ["aim", "lim"], ["t2"])
            tt(kre, t1, t2, ALU.add, ["t1", "t2"], ["kre"])
            tt(kre, kre, den, ALU.mult, ["kre", "den"], ["kre"])
            tt(t1, aim, lre, ALU.mult, ["aim", "lre"], ["t1"])
            tt(t2, am1, lim, ALU.mult, ["am1", "lim"], ["t2"])
            tt(kim, t1, t2, ALU.subtract, ["t1", "t2"], ["kim"])
            tt(kim, kim, den, ALU.mult, ["kim", "den"], ["kim"])
            import os
            BSTOP = int(os.environ.get("BSTOP", "99"))
            Bz = AR.alloc(64 * 128, BF16).rearrange("p (g c) -> p g c", g=64)
            Cz = AR.alloc(64 * 128, BF16).rearrange("p (g c) -> p g c", g=64)
            pw_re = AR.alloc(nsteps_b * 64).rearrange("p (k g) -> p k g", k=nsteps_b)
            pw_im = AR.alloc(nsteps_b * 64).rearrange("p (k g) -> p k g", k=nsteps_b)
            NM = 3 * ((nsteps_b) // 2) + (nsteps_b % 2)
            c0 = AR.alloc(NM * 64).rearrange("p (k g) -> p k g", k=NM)
            c1 = AR.alloc(NM * 64).rearrange("p (k g) -> p k g", k=NM)
            PR = AR.alloc(NM * 64).rearrange("p (k g) -> p k g", k=NM)
            PI = AR.alloc(NM * 64).rearrange("p (k g) -> p k g", k=NM)
            mark = AR.off
            bre = AR.alloc(1024).rearrange("p (g h) -> p g h", g=64)
            bim = AR.alloc(1024).rearrange("p (g h) -> p g h", g=64)
            for hlf in range(2):
                dma("sync", bre[hlf * 64:(hlf + 1) * 64], b_re.rearrange("d g p h -> p (d g) h"), "d_bre", (), ["bre"])
                dma("sync", bim[hlf * 64:(hlf + 1) * 64], b_im.rearrange("d g p h -> p (d g) h"), "d_bim", (), ["bim"])
            bb = AR.alloc(1024).rearrange("p (g h) -> p g h", g=64)
            tb1 = AR.alloc(1024).rearrange("p (g h) -> p g h", g=64)
            kre_b = kre.unsqueeze(2).to_broadcast([128, 64, 16])
            kim_b = kim.unsqueeze(2).to_broadcast([128, 64, 16])
            tt(bb[0:64], bre[0:64], kre_b[0:64], ALU.mult, ["bre", "kre"], ["bb"])
            tt(tb1[0:64], bim[0:64], kim_b[0:64], ALU.mult, ["bim", "kim"], ["tb1"])
            tt(bb[0:64], bb[0:64], tb1[0:64], ALU.subtract, ["bb", "tb1"], ["bb"])
            tt(bb[64:128], bim[64:128], kre_b[64:128], ALU.mult, ["bim", "kre"], ["bb"])
            tt(tb1[64:128], bre[64:128], kim_b[64:128], ALU.mult, ["bre", "kim"], ["tb1"])
            tt(bb[64:128], bb[64:128], tb1[64:128], ALU.add, ["bb", "tb1"], ["bb"])
            Sg = AR.alloc(128)
            for gd in range(64):
                sl = gd % 8
                mset(Sg, 0.0, ["Sg"])
                cp(Sg[:, sl * 16:(sl + 1) * 16], bb[:, gd, :], ["bb"], ["Sg"])
                pb, pk = psb()
                tr(pb[:, 0:128], Sg, ident_f, ["Sg", "ident_f"], [pk])
                cp(Bz[:, gd, :], pb[:, 0:128], [pk], ["Bz"], eng="scalar")
            cst_ = AR.alloc(64 * 128, BF16).rearrange("p (g c) -> p g c", g=64)
            mset(cst_, 0.0, ["cst_"])
            for sl in range(8):
                dma("gpsimd", cst_[sl * 16:(sl + 1) * 16].rearrange("p (d q s) c -> p d q s c", d=2, q=4, s=8)[:, :, :, sl, 0:64],
                    c_re.rearrange("d (q s) h p -> s h d q p", s=8)[sl], "d_cst", (), ["cst_"])
                dma("gpsimd", cst_[sl * 16:(sl + 1) * 16].rearrange("p (d q s) c -> p d q s c", d=2, q=4, s=8)[:, :, :, sl, 64:128],
                    c_im.rearrange("d (q s) h p -> s h d q p", s=8)[sl], "d_cst", (), ["cst_"])
            ts(cst_[:, :, 64:128], cst_[:, :, 64:128], -1.0, ALU.mult, ["cst_"], ["cst_"])
            for gd in range(64):
                pb, pk = psb()
                pbb = pb[:, 0:64].bitcast(BF16)
                tr(pbb, cst_[:, gd, :], ident_b, ["cst_", "ident_b"], [pk])
                cp(Cz[:, gd, :], pbb, [pk], ["Cz"], eng="scalar")
            cp(pw_re[:, 0, :], are, ["are"], ["pw"])
            cp(pw_im[:, 0, :], aim, ["aim"], ["pw"])
            for k in range(1, nsteps_b):
                tt(t1, pw_re[:, k - 1, :], pw_re[:, k - 1, :], ALU.mult, ["pw"], ["t1"])
                tt(t2, pw_im[:, k - 1, :], pw_im[:, k - 1, :], ALU.mult, ["pw"], ["t2"])
                tt(pw_re[:, k, :], t1, t2, ALU.subtract, ["t1", "t2"], ["pw"])
                tt(t1, pw_re[:, k - 1, :], pw_im[:, k - 1, :], ALU.mult, ["pw"], ["t1"])
                ts(pw_im[:, k, :], t1, 2.0, ALU.mult, ["t1"], ["pw"])
            steps_all = []
            w_ = 1
            while w_ < S:
                if w_ * 4 <= S:
                    steps_all.append((w_, [1, 2, 3])); w_ *= 4
                else:
                    steps_all.append((w_, [1])); w_ *= 2
            midx = {}
            for (sh_, ms_) in steps_all:
                for m_ in ms_:
                    midx[(sh_, m_)] = len(midx)
            for (sh_, m_), ix in midx.items():
                k_ = sh_.bit_length() - 1
                if m_ == 1:
                    cp(PR[:, ix, :], pw_re[:, k_, :], ["pw"], ["PRI"])
                    cp(PI[:, ix, :], pw_im[:, k_, :], ["pw"], ["PRI"])
                elif m_ == 2:
                    cp(PR[:, ix, :], pw_re[:, k_ + 1, :], ["pw"], ["PRI"])
                    cp(PI[:, ix, :], pw_im[:, k_ + 1, :], ["pw"], ["PRI"])
                else:
                    tt(t1, pw_re[:, k_, :], pw_re[:, k_ + 1, :], ALU.mult, ["pw"], ["t1"])
                    tt(t2, pw_im[:, k_, :], pw_im[:, k_ + 1, :], ALU.mult, ["pw"], ["t2"])
                    tt(PR[:, ix, :], t1, t2, ALU.subtract, ["t1", "t2"], ["PRI"])
                    tt(t1, pw_re[:, k_, :], pw_im[:, k_ + 1, :], ALU.mult, ["pw"], ["t1"])
                    tt(t2, pw_im[:, k_, :], pw_re[:, k_ + 1, :], ALU.mult, ["pw"], ["t2"])
                    tt(PI[:, ix, :], t1, t2, ALU.add, ["t1", "t2"], ["PRI"])
            cp(c0[0:64], PR[0:64], ["PRI"], ["c01"])
            ts(c0[64:128], PI[64:128], -1.0, ALU.mult, ["PRI"], ["c01"])
            cp(c1[0:64], PI[0:64], ["PRI"], ["c01"])
            cp(c1[64:128], PR[64:128], ["PRI"], ["c01"])
            if BSTOP <= 1:
                j_range = []
            else:
                j_range = range(4)
            P.barrier()
            AR.off = mark
            ut = AR.alloc(S, BF16)
            Xf = AR.alloc(S)
            Xb = AR.alloc(S, BF16)
            ysum = AR.alloc(NO)
            ysb = AR.alloc(NO, BF16)
            Mk = [AR.alloc(NM * 128, BF16).rearrange("p (k c) -> p k c", k=NM) for _ in range(2)]
            gi = 0
            for j in j_range:
                dma("sync", ut, uT[j * 128:(j + 1) * 128, :], "d_ut", (), ["ut"])
                first = True
                for g in range(j * 8, j * 8 + 8):
                    for d in range(2):
                        gd = d * 32 + g
                        ncol = NO if d == 0 else S
                        steps = []
                        for (sh_, ms_) in steps_all:
                            if 4 * sh_ <= ncol:
                                steps.append((sh_, [m_ for m_ in ms_]))
                            elif 2 * sh_ <= ncol:
                                steps.append((sh_, [1]))
                        nblk = ncol // 512
                        mk = Mk[gi % 2]
                        mkey = "Mk%d" % (gi % 2)
                        gi += 1
                        for (sh_, ms_) in steps:
                            for m_ in ms_:
                                ix = midx[(sh_, m_)]
                                ts(mk[:, ix, 0:64], i2c[:, 0:64], c0[:, ix, gd:gd + 1], ALU.mult, ["i2c", "c01"], [mkey], eng="gpsimd")
                                ts(mk[:, ix, 64:128], i2c[:, 64:128], c1[:, ix, gd:gd + 1], ALU.mult, ["i2c", "c01"], [mkey], eng="gpsimd")
                        for b in range(nblk):
                            pb, pk = psb()
                            mm(pb[:, :], Bz[:, gd, :], ut[:, b * 512:(b + 1) * 512], True, True, ["Bz", "ut"], [pk])
                            cp(Xf[:, b * 512:(b + 1) * 512], pb[:, :], [pk], [("Xf", b)], eng="scalar")
                            cp(Xb[:, b * 512:(b + 1) * 512], pb[:, :], [pk], [("Xb", b)])
                        for (sh_, ms_) in steps:
                            order = range(nblk - 1, -1, -1) if d == 0 else range(nblk)
                            for b in order:
                                rngs = []
                                for m_ in ms_:
                                    off = m_ * sh_
                                    if d == 0:
                                        t0 = max(b * 512, off); t1_ = (b + 1) * 512
                                        s0 = t0 - off
                                    else:
                                        t0 = b * 512; t1_ = min((b + 1) * 512, ncol - off)
                                        s0 = t0 + off
                                    if t0 < t1_:
                                        rngs.append((m_, t0, t1_, s0))
                                if not rngs:
                                    continue
                                _, T0, T1_, _ = rngs[0]
                                pb, pk = psb()
                                for ri, (m_, t0, t1_, s0) in enumerate(rngs):
                                    n = t1_ - t0
                                    sblks = sorted(set([s0 // 512, (s0 + n - 1) // 512]))
                                    mm(pb[:, t0 - T0:t0 - T0 + n], mk[:, midx[(sh_, m_)], :], Xb[:, s0:s0 + n], ri == 0, ri == len(rngs) - 1,
                                       [mkey] + [("Xb", q) for q in sblks], [pk])
                                N = T1_ - T0
                                tt(Xf[:, T0:T1_], Xf[:, T0:T1_], pb[:, 0:N], ALU.add, [pk, ("Xf", b)], [("Xf", b)])
                                cp(Xb[:, T0:T1_], Xf[:, T0:T1_], [("Xf", b)], [("Xb", b)], eng="scalar")
                        for b in range(NBO):
                            pb, pk = psb()
                            mm(pb[:, :], Cz[:, gd, :], Xb[:, b * 512:(b + 1) * 512], True, True, ["Cz", ("Xb", b)], [pk])
                            if first:
                                cp(ysum[:, b * 512:(b + 1) * 512], pb[:, :], [pk], [("ys", b)])
                            else:
                                tt(ysum[:, b * 512:(b + 1) * 512], ysum[:, b * 512:(b + 1) * 512], pb[:, :], ALU.add, [pk, ("ys", b)], [("ys", b)])
                        first = False
                for b in range(NBO):
                    stt(ysum[:, b * 512:(b + 1) * 512], ut[:, b * 512:(b + 1) * 512], dsk[:, j:j + 1], ysum[:, b * 512:(b + 1) * 512],
                        ALU.mult, ALU.add, ["ut", "dsk", ("ys", b)], [("ys", b)])
                    act(ysb[:, b * 512:(b + 1) * 512], ysum[:, b * 512:(b + 1) * 512], AF.Gelu_apprx_tanh, [("ys", b)], [("ysb", b)])
                dma("sync", ysT[j * 128:(j + 1) * 128, :], ysb, "st_ysb", [("ysb", b) for b in range(NBO)], ["ysT"])
            P.barrier()

        if "C" in phases:
            AR.reset()
            scale = 96.0 ** -0.5
            stage = AR.alloc(1024)
            Wq = AR.alloc(2 * 768, BF16).rearrange("p (k c) -> p k c", k=2)
            Wqr = AR.alloc(2 * 768, BF16).rearrange("p (k c) -> p k c", k=2)
            Wkv = AR.alloc(1024, BF16).rearrange("p (k c) -> p k c", k=1)
            load_scaled_w(Wq, w_q_b, 2, 768, None, None, "Wq", stage, "stage")
            load_scaled_w(Wkv, w_kv_b, 1, 1024, None, None, "Wkv", stage, "stage")
            mset(Wqr, 0.0, ["Wqr"])
            Wq4 = Wq.rearrange("p k (h c) -> p k h c", h=8)
            Wqr4 = Wqr.rearrange("p k (h c) -> p k h c", h=8)
            for k in range(2):
                ts(Wqr4[:, k, :, 64:80], Wq4[:, k, :, 80:96], -1.0, ALU.mult, ["Wq"], ["Wqr"])
                cp(Wqr4[:, k, :, 80:96], Wq4[:, k, :, 64:80], ["Wq"], ["Wqr"])
            lat = AR.alloc(S, BF16)
            hq = AR.alloc(2 * NO, BF16).rearrange("p (k t) -> p k t", k=2)
            dma("sync", lat, latT, "d_lat", ["latT"], ["lat"])
            dma("sync", hq, hqT.rearrange("(k p) t -> p k t", p=128), "d_hq", ["hqT"], ["hq"])
            cq = AR.alloc(NO); sq_ = AR.alloc(NO)
            dma("scalar", cq[64:96, :], cosT[:, 0:NO], "d_cq", ["ropeC"], ["cq"])
            dma("scalar", sq_[64:96, :], sinT[:, 0:NO], "d_sq", ["ropeS"], ["sq_"])
            KT = [AR.alloc(S, BF16) for _ in range(2)]
            for s in range(2):
                dma("sync", KT[s][64:96, :], krT, "d_KT%d" % s, ["krT"], [("KT", s)])
                mset(KT[s][96:97, :], 1.0, [("KT", s)])
            V = [AR.alloc(NKT * 65, BF16).rearrange("p (k c) -> p k c", k=NKT) for _ in range(2)]
            for s in range(2):
                mset(V[s][:, :, 64:65], 1.0, [("V", s)])
            QT = [AR.alloc(NO, BF16) for _ in range(2)]
            sqk = [AR.alloc(512, BF16) for _ in range(2)]
            m16 = AR.alloc(max(NB, 8)); kmx = AR.alloc(1); nkm = AR.alloc(1)
            t1f = [AR.alloc(512) for _ in range(2)]
            t2f = [AR.alloc(512) for _ in range(2)]
            Pt = [AR.alloc(512, BF16) for _ in range(4)]
            osb = [AR.alloc(512) for _ in range(2)]
            obf = [AR.alloc(512, BF16) for _ in range(2)]
            pi = 0
            for h in range(8):
                s = h % 2
                ktk, vk, qk = ("KT", s), ("V", s), ("QT", s)
                for b in range(NB):
                    pb, pk = psb()
                    mm(pb[0:64, :], Wkv[:, 0, h * 128:h * 128 + 64], lat[:, b * 512:(b + 1) * 512], True, True, ["Wkv", "lat"], [pk])
                    cp(KT[s][0:64, b * 512:(b + 1) * 512], pb[0:64, :], [pk], [ktk], eng="scalar")
                    act(sqk[b % 2][0:96, :], KT[s][0:96, b * 512:(b + 1) * 512], AF.Square, [ktk], [("sqk", b % 2)])
                    pb2, pk2 = psb()
                    mm(pb2[:, :], ones_b[0:96, :], sqk[b % 2][0:96, :], True, True, [("sqk", b % 2), "ones_b"], [pk2])
                    P.op("vector", (lambda pb2, b: lambda e: e.tensor_reduce(out=m16[:, b:b + 1], in_=pb2[:, :], axis=AX.X, op=ALU.max))(pb2, b), [pk2], ["m16"])
                P.op("vector", lambda e: e.tensor_reduce(out=kmx, in_=m16[:, 0:NB], axis=AX.X, op=ALU.max), ["m16"], ["kmx"])
                act(kmx, kmx, AF.Sqrt, ["kmx"], ["kmx"])
                ts(nkm, kmx, -1.0, ALU.mult, ["kmx"], ["nkm"])
                for k0 in range(0, NKT, 8):
                    pb, pk = psb()
                    for kk_ in range(8):
                        kt = k0 + kk_
                        mm(pb[:, kk_ * 64:(kk_ + 1) * 64], lat[:, kt * 128:(kt + 1) * 128], Wkv[:, 0, h * 128 + 64:h * 128 + 128], True, True, ["Wkv", "lat"], [pk])
                    cp(V[s][:, k0:k0 + 8, 0:64], pb[:, :].rearrange("p (k c) -> p k c", k=8), [pk], [vk])
                for qb in range(NBO):
                    pa, pka = psb()
                    pr, pkr = psb()
                    for k in range(2):
                        mm(pa[0:96, :], Wq[:, k, h * 96:(h + 1) * 96], hq[:, k, qb * 512:(qb + 1) * 512], k == 0, k == 1, ["Wq", "hq"], [pka])
                    for k in range(2):
                        mm(pr[0:96, :], Wqr[:, k, h * 96:(h + 1) * 96], hq[:, k, qb * 512:(qb + 1) * 512], k == 0, k == 1, ["Wqr", "hq"], [pkr])
                    cp(QT[s][0:64, qb * 512:(qb + 1) * 512], pa[0:64, :], [pka], [qk], eng="scalar")
                    a1, a2 = t1f[qb % 2], t2f[qb % 2]
                    tt(a1[64:96, :], pa[64:96, :], cq[64:96, qb * 512:(qb + 1) * 512], ALU.mult, [pka, "cq"], [("t1f", qb % 2)])
                    tt(a2[64:96, :], pr[64:96, :], sq_[64:96, qb * 512:(qb + 1) * 512], ALU.mult, [pkr, "sq_"], [("t2f", qb % 2)])
                    tt(QT[s][64:96, qb * 512:(qb + 1) * 512], a1[64:96, :], a2[64:96, :], ALU.add, [("t1f", qb % 2), ("t2f", qb % 2)], [qk])
                    act(sqk[qb % 2][0:96, :], QT[s][0:96, qb * 512:(qb + 1) * 512], AF.Square, [qk], [("sqk", qb % 2)])
                    pn, pkn = psb()
                    mm(pn[:, :], ones_b[0:96, :], sqk[qb % 2][0:96, :], True, True, [("sqk", qb % 2), "ones_b"], [pkn])
                    act(a1[96:97, :], pn[96:97, :], AF.Sqrt, [pkn], [("t1f", qb % 2)])
                    ts(QT[s][96:97, qb * 512:(qb + 1) * 512], a1[96:97, :], nkm[96:97, 0:1], ALU.mult, [("t1f", qb % 2), "nkm"], [qk])
                for qb in range(NBO):
                    po, pko = psb()
                    prev = []
                    for kt in range(NKT):
                        pS, pkS = psb()
                        if pkS == pko:
                            pS, pkS = psb()
                        mm(pS[:, :], KT[s][0:97, kt * 128:(kt + 1) * 128], QT[s][0:97, qb * 512:(qb + 1) * 512], True, True, [ktk, qk], [pkS])
                        pt = Pt[pi % 4]; ptk = ("Pt", pi % 4); pi += 1
                        act(pt, pS[:, :], AF.Exp, [pkS], [ptk], scale=scale)
                        prev.append((kt, pt, ptk))
                        if len(prev) > 2:
                            k_, p_, pk_ = prev.pop(0)
                            mm(po[0:65, :], V[s][:, k_, 0:65], p_, k_ == 0, k_ == NKT - 1, [vk, pk_], [pko])
                    for (k_, p_, pk_) in prev:
                        mm(po[0:65, :], V[s][:, k_, 0:65], p_, k_ == 0, k_ == NKT - 1, [vk, pk_], [pko])
                    o = osb[qb % 2]; ok = ("osb", qb % 2)
                    cp(o[0:65, :], po[0:65, :], [pko], [ok])
                    recip(o[64:65, :], o[64:65, :], [ok], [ok])
                    pr, pkr = psb()
                    mm(pr[0:64, :], ones_f[64:65, 0:64], o[64:65, :], True, True, [ok, "ones_f"], [pkr])
                    ob_ = obf[qb % 2]; obk = ("obf", qb % 2)
                    tt(ob_[0:64, :], o[0:64, :], pr[0:64, :], ALU.mult, [ok, pkr], [obk])
                    dma("sync", oT[h, :, qb * 512:(qb + 1) * 512], ob_[0:64, :], "st_obf%d" % (qb % 2), [obk], ["oT"])
            P.barrier()

        x1T = dscr("x1T", [1024, NO], F32)
        if "D" in phases:
            AR.reset()
            stage = AR.alloc(1024)
            Wg = AR.alloc(8 * 2048, BF16).rearrange("p (k c) -> p k c", k=8)
            Wglu = AR.alloc(4 * 1024, BF16).rearrange("p (k c) -> p k c", k=4)
            Wos = AR.alloc(4 * 1024, BF16).rearrange("p (k c) -> p k c", k=4)
            Woa = AR.alloc(8 * 1024, BF16).rearrange("p (k c) -> p k c", k=8)
            Wout = AR.alloc(8 * 1024, BF16).rearrange("p (k c) -> p k c", k=8)
            load_scaled_w(Wg, w_in, 8, 2048, gmix, "gmix", "Wg", stage, "stage", c0=928)
            load_scaled_w(Wglu, w_glu, 4, 1024, None, None, "Wglu", stage, "stage")
            load_scaled_w(Wos, w_o_ssm, 4, 1024, None, None, "Wos", stage, "stage")
            for h in range(8):
                dma("sync", stage[0:64, :], w_o_attn[h * 64:(h + 1) * 64, :], "d_stage", (), ["stage"])
                cp(Woa[0:64, h, :], stage[0:64, :], ["stage"], ["Woa"])
            load_scaled_w(Wout, w_out, 8, 1024, None, None, "Wout", stage, "stage")
            xf = AR.alloc(8 * 512).rearrange("p (k t) -> p k t", k=8)
            xb = AR.alloc(8 * 512, BF16).rearrange("p (k t) -> p k t", k=8)
            sq = AR.alloc(8 * 512, BF16).rearrange("p (k t) -> p k t", k=8)
            rstd = AR.alloc(512)
            ysb_ = AR.alloc(4 * 512, BF16).rearrange("p (k t) -> p k t", k=4)
            glu = AR.alloc(4 * 512, BF16).rearrange("p (k t) -> p k t", k=4)
            otb = AR.alloc(8 * 512, BF16).rearrange("p (k t) -> p k t", k=8)
            sg = [AR.alloc(512) for _ in range(2)]
            mixed = AR.alloc(8 * 512, BF16).rearrange("p (k t) -> p k t", k=8)
            x1 = AR.alloc(8 * 512).rearrange("p (k t) -> p k t", k=8)
            import os
            DSTOP = int(os.environ.get("DSTOP", "99"))
            for blk in range(NBO if DSTOP > 0 else 0):
                tg = "D"
                x_block(blk, xf, xb, sq, rstd, tg)
                dma("scalar", ysb_, ysT[:, blk * 512:(blk + 1) * 512].rearrange("(k p) t -> p k t", p=128), "d_ysb", ["ysT"], ["ysb_"])
                dma("scalar", otb[0:64], oT[:, :, blk * 512:(blk + 1) * 512].rearrange("h p t -> p h t"), "d_otb", ["oT"], ["otb"])
                if DSTOP <= 1:
                    continue
                for j in range(4):
                    pv, pkv = psb()
                    pg, pkg = psb()
                    for k in range(4):
                        mm(pv[:, :], Wglu[:, k, j * 128:(j + 1) * 128], ysb_[:, k, :], k == 0, k == 3, ["Wglu", "ysb_"], [pkv])
                    for k in range(4):
                        mm(pg[:, :], Wglu[:, k, 512 + j * 128:512 + (j + 1) * 128], ysb_[:, k, :], k == 0, k == 3, ["Wglu", "ysb_"], [pkg])
                    act(sg[0], pg[:, :], AF.Sigmoid, [pkg], ["sg0"])
                    tt(glu[:, j, :], pv[:, :], sg[0], ALU.mult, [pkv, "sg0"], ["glu"])
                if DSTOP <= 2:
                    continue
                for j in range(8):
                    pa, pka = psb(); ps_, pks = psb(); p0, pk0 = psb(); p1, pk1 = psb()
                    DVAR = int(os.environ.get("DVAR", "0"))
                    hs = [0, 2, 4, 6] if DVAR == 1 else list(range(8))
                    for h in hs:
                        mm(pa[:, :], Woa[0:64, h, j * 128:(j + 1) * 128], otb[0:64, h, :], h == hs[0], h == hs[-1], ["Woa", "otb"], [pka])
                    for k in range(4):
                        mm(ps_[:, :], Wos[:, k, j * 128:(j + 1) * 128], glu[:, k, :], k == 0, k == 3, ["Wos", "glu"], [pks])
                    for k in range(8):
                        mm(p0[:, :], Wg[:, k, j * 128:(j + 1) * 128], xb[:, k, :], k == 0, k == 7, ["Wg", "xb" + tg], [pk0])
                    for k in range(8):
                        mm(p1[:, :], Wg[:, k, 1024 + j * 128:1024 + (j + 1) * 128], xb[:, k, :], k == 0, k == 7, ["Wg", "xb" + tg], [pk1])
                    tt(sg[0], p0[:, :], rstd, ALU.mult, [pk0, "rstd" + tg], ["sg0"])
                    act(sg[0], sg[0], AF.Sigmoid, ["sg0"], ["sg0"])
                    tt(sg[1], p1[:, :], rstd, ALU.mult, [pk1, "rstd" + tg], ["sg1"])
                    act(sg[1], sg[1], AF.Sigmoid, ["sg1"], ["sg1"])
                    tt(sg[0], sg[0], pa[:, :], ALU.mult, ["sg0", pka], ["sg0"])
                    tt(sg[1], sg[1], ps_[:, :], ALU.mult, ["sg1", pks], ["sg1"])
                    tt(mixed[:, j, :], sg[0], sg[1], ALU.add, ["sg0", "sg1"], ["mixed"])
                if DSTOP <= 3:
                    continue
                for j in range(8):
                    pb, pk = psb()
                    for k in range(8):
                        mm(pb[:, :], Wout[:, k, j * 128:(j + 1) * 128], mixed[:, k, :], k == 0, k == 7, ["Wout", "mixed"], [pk])
                    tt(x1[:, j, :], xf[:, j, :], pb[:, :], ALU.add, ["xf" + tg, pk], ["x1"])
                dma("sync", x1T[:, blk * 512:(blk + 1) * 512].rearrange("(k p) t -> p k t", p=128), x1, "st_x1", ["x1"], ["x1T"])
            P.barrier()

        if "E" in phases:
            AR.reset()
            DELTA = 2e-5
            Kbd = AR.alloc(8 * 256, BF16).rearrange("p (h c) -> p h c", h=8)
            x1 = AR.alloc(8 * 512).rearrange("p (k t) -> p k t", k=8)
            rstd = AR.alloc(512)
            h2 = AR.alloc(8 * 512, BF16).rearrange("p (k t) -> p k t", k=8)
            sc = AR.alloc(4 * 2048).rearrange("p (t c) -> p t c", t=4)
            wk = AR.alloc(256)
            tv = AR.alloc(16 * 16).rearrange("p (g k) -> p g k", g=16)
            best = AR.alloc(8 * 16).rearrange("p (h k) -> p h k", h=8)
            eb = AR.alloc(8 * 16).rearrange("p (h k) -> p h k", h=8)
            zs = AR.alloc(8)
            lnz = AR.alloc(4 * 8).rearrange("p (t h) -> p t h", t=4)
            thr = AR.alloc(4 * 8).rearrange("p (t h) -> p t h", t=4)
            wd_off = AR.off
            WdT = [AR.alloc(8 * 512, BF16).rearrange("p (k c) -> p k c", k=8) for _ in range(2)]
            Wu = [AR.alloc(4 * 1024, BF16).rearrange("p (k c) -> p k c", k=4) for _ in range(2)]
            T1f = [AR.alloc(4096) for _ in range(2)]
            T1 = [t.rearrange("p (h a b) -> p h a b", h=8, a=4) for t in T1f]
            T1k = ["T1_0", "T1_1"]
            stage = T1f[0][:, 0:1024]
            cand = T1f[0][:, 0:2048].rearrange("p (h c) -> p h c", h=8)
            Wqy = AR.ap[:, wd_off:wd_off + 4096].bitcast(BF16).rearrange("p (k c) -> p k c", k=8)
            Ef = [AR.alloc(4096, BF16) for _ in range(2)]
            E_ = [t.rearrange("p (h c) -> p h c", h=8) for t in Ef]
            Ek = ["E_0", "E_1"]
            qT = Ef[0].rearrange("p (k t) -> p k t", k=8)
            sq = Ef[1].rearrange("p (k t) -> p k t", k=8)
            Em = AR.alloc(4096, BF16).rearrange("p (h c) -> p h c", h=8)
            gelT = [AR.alloc(512, BF16) for _ in range(3)]
            zT = [AR.alloc(512, BF16) for _ in range(2)]
            zb = [AR.alloc(512, BF16) for _ in range(2)]
            yacc = AR.alloc(4 * 1024).rearrange("p (t c) -> p t c", t=4)
            mset(Kbd, 0.0, ["Kbd"])
            for h in range(8):
                dma("sync", stage[:, 0:128].rearrange("p (n d) -> p n d", n=2), sub_keys[h].rearrange("n k d -> k n d"), "d_stage", (), ["T1_0"])
                pb, pk = psb()
                tr(pb[:, 0:128], stage[:, 0:128], ident_f, ["T1_0", "ident_f"], [pk])
                cp(Kbd[0:64, h, 0:128], pb[0:64, 0:128], [pk], ["Kbd"])
                cp(Kbd[64:128, h, 128:256], pb[64:128, 0:128], [pk], ["Kbd"])
            tg = "E"
            for blk in range(NBO):
                dma("sync", x1, x1T[:, blk * 512:(blk + 1) * 512].rearrange("(k p) t -> p k t", p=128), "d_x1", ["x1T"], ["x1"])
                dma("gpsimd", Wqy, w_query.rearrange("(k p) c -> p k c", p=128), "d_Wqy", (), [("WdT", 0), ("WdT", 1)])
                act(sq, x1, AF.Square, ["x1"], ["E_1"])
                norm_rstd([sq[:, k, :] for k in range(8)], 1024.0, rstd, ["E_1"], "rstd" + tg)
                for k in range(8):
                    stt(h2[:, k, :], x1[:, k, :], gffn[:, k:k + 1], rstd, ALU.mult, ALU.mult, ["x1", "gffn", "rstd" + tg], ["h2"])
                for j in range(8):
                    pb, pk = psb()
                    for k in range(8):
                        mm(pb[:, :], Wqy[:, k, j * 128:(j + 1) * 128], h2[:, k, :], k == 0, k == 7, [("WdT", 0), ("WdT", 1), "h2"], [pk])
                    cp(qT[:, j, :], pb[:, :], [pk], ["E_0"], eng="scalar")
                for t_ in range(4):
                    for hp in range(4):
                        pb, pk = psb()
                        for hh in range(2):
                            h = hp * 2 + hh
                            mm(pb[:, hh * 256:(hh + 1) * 256], qT[:, h, t_ * 128:(t_ + 1) * 128], Kbd[:, h, :], True, True, ["E_0", "Kbd"], [pk])
                        cp(sc[:, t_, hp * 512:(hp + 1) * 512], pb[:, :], [pk], ["sc"], eng="scalar")
                    sc3 = sc[:, t_, :].rearrange("p (g k) -> p g k", g=16)
                    for g in range(16):
                        P.op("vector", (lambda g, sc3: lambda e: e.max(out=tv[:, g, 0:8], in_=sc3[:, g, :]))(g, sc3), ["sc"], ["tv"])
                        P.op("vector", (lambda g, sc3: lambda e: e.match_replace(out=wk[:, 0:128], in_to_replace=tv[:, g, 0:8], in_values=sc3[:, g, :], imm_value=-1e30))(g, sc3), ["sc", "tv"], ["wk"])
                        P.op("vector", (lambda g: lambda e: e.max(out=tv[:, g, 8:16], in_=wk[:, 0:128]))(g), ["wk"], ["tv"])
                    tv4 = tv.rearrange("p (h n) k -> p h n k", n=2)
                    tt(cand.rearrange("p h (a b) -> p h a b", a=16), tv4[:, :, 0, :].unsqueeze(3).to_broadcast([128, 8, 16, 16]),
                       tv4[:, :, 1, :].unsqueeze(2).to_broadcast([128, 8, 16, 16]), ALU.add, ["tv"], ["T1_0"])
                    for h in range(8):
                        P.op("vector", (lambda h: lambda e: e.max(out=best[:, h, 0:8], in_=cand[:, h, :]))(h), ["T1_0"], ["best"])
                        P.op("vector", (lambda h: lambda e: e.match_replace(out=wk[:, 0:256], in_to_replace=best[:, h, 0:8], in_values=cand[:, h, :], imm_value=-1e30))(h), ["T1_0", "best"], ["wk"])
                        P.op("vector", (lambda h: lambda e: e.max(out=best[:, h, 8:16], in_=wk[:, 0:256]))(h), ["wk"], ["best"])
                    tt(eb, best, best[:, :, 15:16].to_broadcast([128, 8, 16]), ALU.subtract, ["best"], ["eb"])
                    act(eb, eb, AF.Exp, ["eb"], ["eb"])
                    P.op("vector", lambda e: e.tensor_reduce(out=zs, in_=eb, axis=AX.X, op=ALU.add), ["eb"], ["zs"])
                    act(lnz[:, t_, :], zs, AF.Ln, ["zs"], ["lnz"])
                    sc4 = sc[:, t_, :].rearrange("p (h n k) -> p h n k", h=8, n=2)
                    tt(sc4[:, :, 0, :], sc4[:, :, 0, :], best[:, :, 15:16].to_broadcast([128, 8, 128]), ALU.subtract, ["sc", "best"], ["sc"])
                    ts(sc4[:, :, 0, :], sc4[:, :, 0, :], DELTA, ALU.add, ["sc"], ["sc"])
                    ts(thr[:, t_, :], lnz[:, t_, :], -1.0, ALU.mult, ["lnz"], ["thr"], s2=-DELTA, op1=ALU.add)
                    mset(yacc[:, t_, :], 0.0, ["yacc"])
                its = [(ec, t_) for ec in range(32) for t_ in range(4)]

                NIT = len(its)
                PA = [(pbs[0], pbk[0]), (pbs[1], pbk[1])]
                PG = [(pbs[2], pbk[2]), (pbs[3], pbk[3])]
                PZ = [(pbs[4], pbk[4]), (pbs[5], pbk[5])]
                PY = [(pbs[6], pbk[6]), (pbs[7], pbk[7])]

                def opA(n):
                    ec, t_ = its[n]
                    s = ec % 2
                    if t_ == 0:
                        dma("gpsimd", WdT[s], w_downT[:, ec * 512:(ec + 1) * 512].rearrange("(k p) c -> p k c", p=128), "d_WdT%d" % s, (), [("WdT", s)])
                        dma("gpsimd", Wu[s], w_up[ec * 512:(ec + 1) * 512, :].rearrange("(k p) c -> p k c", p=128), "d_Wu%d" % s, (), [("Wu", s)])
                    pa, pka = PA[n % 2]
                    for k in range(8):
                        mm(pa[:, :], h2[:, k, t_ * 128:(t_ + 1) * 128], WdT[s][:, k, :], k == 0, k == 7, ["h2", ("WdT", s)], [pka])

                def opGelu(n):
                    pa, pka = PA[n % 2]
                    act(gelT[n % 3], pa[:, :], AF.Gelu_apprx_tanh, [pka], [("gelT", n % 3)])

                def opT1(n):
                    ec, t_ = its[n]
                    i = n % 2
                    sc4 = sc[:, t_, :].rearrange("p (h n k) -> p h n k", h=8, n=2)
                    tt(T1[i], sc4[:, :, 0, ec * 4:(ec + 1) * 4].unsqueeze(3).to_broadcast([128, 8, 4, 128]),
                       sc4[:, :, 1, :].unsqueeze(2).to_broadcast([128, 8, 4, 128]), ALU.add, ["sc"], [T1k[i]])

                def opExp(n):
                    ec, t_ = its[n]
                    i = n % 2
                    T1h = T1f[i].rearrange("p (h c) -> p h c", h=8)
                    for h in range(8):
                        act(E_[i][:, h, :], T1h[:, h, :], AF.Exp, [T1k[i], "thr"], [Ek[i]], bias=thr[:, t_, h:h + 1])

                def opSTT(n):
                    i = n % 2
                    stt(Em.rearrange("p h c -> p (h c)"), T1f[i], 0.0, Ef[i], ALU.is_ge, ALU.mult, [T1k[i], Ek[i]], [("Em", h) for h in range(8)])

                def opG(n):
                    pg, pkg = PG[n % 2]
                    for h in range(8):
                        mm(pg[:, :], ident_b, Em[:, h, :], h == 0, h == 7, [("Em", h), "ident_b"], [pkg])

                def opZb(n):
                    pg, pkg = PG[n % 2]
                    tt(zb[n % 2], gelT[n % 3], pg[:, :], ALU.mult, [("gelT", n % 3), pkg], [("zb", n % 2)])

                def opTr(n):
                    pz, pkz = PZ[n % 2]
                    pzb = pz[:, 0:256].bitcast(BF16).rearrange("p (q t) -> p q t", q=4)
                    for q in range(4):
                        tr(pzb[:, q, :], zb[n % 2][:, q * 128:(q + 1) * 128], ident_b, [("zb", n % 2), "ident_b"], [pkz])

                def opEvac(n):
                    pz, pkz = PZ[n % 2]
                    pzb = pz[:, 0:256].bitcast(BF16).rearrange("p (q t) -> p q t", q=4)
                    cp(zT[n % 2].rearrange("p (q t) -> p q t", q=4), pzb, [pkz], [("zT", n % 2)], eng="scalar")

                def opY(n):
                    ec, t_ = its[n]
                    s = ec % 2
                    z3 = zT[n % 2].rearrange("p (q t) -> p q t", q=4)
                    for hf in range(2):
                        py, pky = PY[hf]
                        for q in range(4):
                            mm(py[:, :], z3[:, q, :], Wu[s][:, q, hf * 512:(hf + 1) * 512], q == 0, q == 3, [("zT", n % 2), ("Wu", s)], [pky])

                def opYacc(n):
                    ec, t_ = its[n]
                    tt(yacc[:, t_, :], yacc[:, t_, :], pby2[:, :], ALU.add, ["yacc", PY[0][1], PY[1][1]], ["yacc"])

                def ok(n):
                    return 0 <= n < NIT

                for i in range(NIT + 3):
                    if ok(i):
                        opA(i); opGelu(i); opT1(i); opExp(i)
                    if ok(i - 1):
                        opSTT(i - 1)
                    if ok(i - 3):
                        opY(i - 3); opYacc(i - 3)
                    if ok(i - 1):
                        opG(i - 1)
                    if ok(i - 2):
                        opZb(i - 2); opTr(i - 2); opEvac(i - 2)
                for t_ in range(4):
                    for j in range(8):
                        pb, pk = psb()
                        tr(pb[:, 0:128], yacc[:, t_, j * 128:(j + 1) * 128], ident_f, ["yacc", "ident_f"], [pk])
                        tt(x1[:, j, t_ * 128:(t_ + 1) * 128], x1[:, j, t_ * 128:(t_ + 1) * 128], pb[:, 0:128], ALU.add, ["x1", pk], ["x1"])
                act(sq, x1, AF.Square, ["x1"], ["E_1"])
                norm_rstd([sq[:, k, :] for k in range(8)], 1024.0, rstd, ["E_1"], "rstd" + tg)
                for k in range(8):
                    stt(x1[:, k, :], x1[:, k, :], gfin[:, k:k + 1], rstd, ALU.mult, ALU.mult, ["x1", "gfin", "rstd" + tg], ["x1"])
                dma("sync", outT[:, blk * 512:(blk + 1) * 512].rearrange("(k p) t -> p k t", p=128), x1, "st_out", ["x1"], ["outT"])
        P.emit(final_wait_keys=[k for k in ("st_out",) if "E" in phases])
    return nc, len(P.ops)


_CACHE = {}


def _consts():
    ident = np.eye(128, dtype=np.float32)
    i64 = np.eye(64, dtype=np.float32)
    i2 = np.block([[i64, i64], [i64, i64]]).astype(np.float32)
    return ident, i2


def make_in_maps(inp, S):
    B = inp["x"].shape[0]
    NO = S // 2
    ident, i2 = _consts()
    w_downT = np.ascontiguousarray(inp["w_down"][0].T)
    maps = []
    for c in range(2 * B):
        b, half = c // 2, c % 2
        xb = inp["x"][b]
        if half:
            xb = xb[::-1]
        pinfo = np.empty((128, 2), dtype=np.float32)
        pinfo[:, 0] = -1.0 if half else 1.0
        pinfo[:, 1] = float(S - 1) if half else 0.0
        dsel = [1, 0] if half else [0, 1]
        m = dict(
            xT=np.ascontiguousarray(xb.T), pinfo=pinfo,
            norm_mix=inp["norm_mix"][0], w_in=inp["w_in"][0], q_a_norm=inp["q_a_norm"][0], w_q_b=inp["w_q_b"][0],
            kv_a_norm=inp["kv_a_norm"][0], w_kv_b=inp["w_kv_b"][0], w_o_attn=inp["w_o_attn"][0],
            lam_re=np.ascontiguousarray(inp["lam_re"][0][dsel]), lam_im=np.ascontiguousarray(inp["lam_im"][0][dsel]),
            log_dt=np.ascontiguousarray(inp["log_dt"][0][dsel].reshape(64)),
            b_re=np.ascontiguousarray(inp["b_re"][0][dsel]), b_im=np.ascontiguousarray(inp["b_im"][0][dsel]),
            c_re=np.ascontiguousarray(inp["c_re"][0][dsel]), c_im=np.ascontiguousarray(inp["c_im"][0][dsel]),
            d_skip=inp["d_skip"][0], w_glu=inp["w_glu"][0], w_o_ssm=inp["w_o_ssm"][0], w_out=inp["w_out"][0],
            norm_ffn=inp["norm_ffn"][0], w_query=inp["w_query"][0], sub_keys=inp["sub_keys"][0],
            w_downT=w_downT, w_up=inp["w_up"][0], final_norm=inp["final_norm"], ident=ident, i2c=i2,
        )
        maps.append({k: np.ascontiguousarray(np.asarray(v, dtype=np.float32)) for k, v in m.items()})
    return maps


def kernel(**inputs):
    inp = {k: np.asarray(v) for k, v in inputs.items()}
    B, S, D = inp["x"].shape
    NO = S // 2
    if S not in _CACHE:
        _CACHE[S] = build(S)[0]
    nc = _CACHE[S]
    maps = make_in_maps(inp, S)
    res = run_bass_kernel_spmd(nc, maps, core_ids=list(range(2 * B)))
    out = np.empty((B, S, D), dtype=np.float32)
    for c in range(2 * B):
        b, half = c // 2, c % 2
        o = np.asarray(res.results[c]["outT"]).T
        if half == 0:
            out[b, :NO] = o
        else:
            out[b, NO:] = o[::-1]
    return out
```
